# Optimizing a Trainium2 kernel written in Bass

```python
import math
import jax, jax.numpy as jnp
from jax import lax
import numpy as np

D_MODEL = 1024
BATCH = 16
SEQ = 4096
DEPTH = 1

PLE_DIM = 256
N_DIFF_HEADS = 4
DIFF_HEAD_DIM = 64
ATTN_WIDTH = N_DIFF_HEADS * 2 * DIFF_HEAD_DIM
POOL_WINDOWS = (2, 4, 8, 16)
N_POOL_GROUPS = len(POOL_WINDOWS)
POOL_GROUP_DIM = 128
POOL_WIDTH = N_POOL_GROUPS * POOL_GROUP_DIM
N_BRANCHES = 2
IN_WIDTH = 3 * ATTN_WIDTH + POOL_WIDTH + N_BRANCHES * D_MODEL
N_EXPERTS = 32
TOP_K = 4
D_FF = D_MODEL
SWIGLU_LIMIT = 7.0
SWIGLU_ALPHA = 1.702
Q_BLOCK = 128
MOE_BLOCK = 128
LN_EPS = 1e-5
DEEPNORM_ALPHA = (2 * DEPTH) ** 0.25
DEEPNORM_BETA = (8 * DEPTH) ** -0.25

kernel_name = 'hybrid_diffattn_pool_moe_deepnorm'

F32 = jnp.float32


def layer_norm(x, g, b):
    xf = x.astype(F32)
    mu = jnp.mean(xf, axis=-1, keepdims=True)
    var = jnp.mean(jnp.square(xf - mu), axis=-1, keepdims=True)
    return ((xf - mu) * lax.rsqrt(var + LN_EPS) * g + b).astype(x.dtype)


def rms_norm(x, g):
    xf = x.astype(F32)
    return (xf * lax.rsqrt(jnp.mean(jnp.square(xf), axis=-1, keepdims=True) + LN_EPS) * g).astype(x.dtype)


def alibi_slopes(n_heads):
    return jnp.asarray(np.array([2.0 ** (-8.0 * (h + 1) / n_heads) for h in range(n_heads)], dtype=np.float32))


def diff_attention(q, k, v, lam):
    Bb, Ss, H = q.shape[0], q.shape[1], q.shape[2]
    nq = Ss // Q_BLOCK
    qb = (q * (DIFF_HEAD_DIM ** -0.5)).reshape(Bb, nq, Q_BLOCK, H, 2, DIFF_HEAD_DIM).transpose(1, 0, 3, 4, 2, 5)
    kt = k.transpose(0, 2, 3, 1, 4)
    vt = v.transpose(0, 2, 1, 3)
    slopes = alibi_slopes(H)
    k_pos = jnp.arange(Ss)

    def block(args):
        q_blk, q_start = args
        s = jnp.einsum('bhmqd,bhmkd->bhmqk', q_blk, kt, preferred_element_type=F32)
        dist = (q_start + jnp.arange(Q_BLOCK))[:, None] - k_pos[None, :]
        bias = jnp.where(dist >= 0, -slopes[:, None, None] * dist.astype(F32), -jnp.inf)
        probs = jax.nn.softmax(s + bias[None, :, None], axis=-1)
        w = probs[:, :, 0] - lam * probs[:, :, 1]
        return jnp.einsum('bhqk,bhkv->bhqv', w.astype(vt.dtype), vt)

    starts = jnp.arange(nq) * Q_BLOCK
    o = lax.map(block, (qb, starts))
    return o.transpose(1, 0, 3, 2, 4).reshape(Bb, Ss, H, 2 * DIFF_HEAD_DIM)


def multiscale_pool(u, w_mix, scale):
    Bb, Ss, _ = u.shape
    ug = u.reshape(Bb, Ss, N_POOL_GROUPS, POOL_GROUP_DIM)
    cs = jnp.cumsum(ug.astype(F32), axis=1)
    pos = jnp.arange(Ss)
    means = []
    for gi, w in enumerate(POOL_WINDOWS):
        c = cs[:, :, gi]
        lagged = jnp.pad(c, ((0, 0), (w, 0), (0, 0)))[:, :Ss]
        count = jnp.minimum(pos + 1, w).astype(F32)[None, :, None]
        means.append((c - lagged) / count)
    pooled = (jnp.stack(means, axis=2) - ug.astype(F32)).astype(u.dtype)
    mixed = jnp.einsum('bsgc,gcd->bsgd', pooled, w_mix).reshape(Bb, Ss, POOL_WIDTH)
    return mixed * scale


def clamped_swiglu(h):
    glu, lin = jnp.split(h, 2, axis=-1)
    glu = jnp.minimum(glu, SWIGLU_LIMIT)
    lin = jnp.clip(lin, -SWIGLU_LIMIT, SWIGLU_LIMIT)
    return glu * jax.nn.sigmoid(SWIGLU_ALPHA * glu) * (lin + 1.0)


def moe(h, w_router, b_router, w_up, b_up, w_down, b_down):
    Bb, Ss, D = h.shape
    T = Bb * Ss
    A = T * TOP_K
    t = h.reshape(T, D)
    logits = (t @ w_router + b_router).astype(F32)
    top_val, top_idx = lax.top_k(logits, TOP_K)
    gate = jax.nn.softmax(top_val, axis=-1).reshape(A)
    flat_e = top_idx.reshape(A)
    flat_tok = jnp.arange(A) // TOP_K
    order = jnp.argsort(flat_e)
    sorted_e = flat_e[order]
    counts = jnp.bincount(flat_e, length=N_EXPERTS)
    padded = ((counts + MOE_BLOCK - 1) // MOE_BLOCK) * MOE_BLOCK
    cum_padded = jnp.cumsum(padded)
    pad_start = cum_padded - padded
    start = jnp.cumsum(counts) - counts
    dest = pad_start[sorted_e] + (jnp.arange(A) - start[sorted_e])
    n_slots = A + N_EXPERTS * MOE_BLOCK
    n_blocks = n_slots // MOE_BLOCK
    slot_tok = jnp.full((n_slots,), T, jnp.int32).at[dest].set(flat_tok[order])
    slot_gate = jnp.zeros((n_slots,), F32).at[dest].set(gate[order])
    block_expert = jnp.minimum(jnp.searchsorted(cum_padded, jnp.arange(n_blocks) * MOE_BLOCK, side='right'), N_EXPERTS - 1)
    t_pad = jnp.concatenate([t, jnp.zeros((1, D), t.dtype)], axis=0)
    xs = t_pad[slot_tok].reshape(n_blocks, MOE_BLOCK, D)

    def expert_block(args):
        xb, e = args
        hb = clamped_swiglu(xb @ w_up[e] + b_up[e])
        return hb @ w_down[e] + b_down[e]

    ys = lax.map(expert_block, (xs, block_expert)).reshape(n_slots, D)
    out = jnp.zeros((T + 1, D), ys.dtype).at[slot_tok].add(ys * slot_gate[:, None].astype(ys.dtype))
    return out[:T].reshape(Bb, Ss, D)


def setup_inputs(seed: int = 0) -> dict:
    key = jax.random.key(seed)
    ks = jax.random.split(key, 32)
    L = DEPTH

    def nrm(k, shape, scale):
        return jax.random.normal(k, shape, F32) * scale

    s_in = D_MODEL ** -0.5
    w_qk = nrm(ks[2], (L, D_MODEL, 2 * ATTN_WIDTH), s_in)
    w_v = nrm(ks[3], (L, D_MODEL, ATTN_WIDTH), s_in * DEEPNORM_BETA)
    w_pool_in = nrm(ks[4], (L, D_MODEL, POOL_WIDTH), s_in)
    w_gate = nrm(ks[5], (L, D_MODEL, N_BRANCHES * D_MODEL), s_in)
    return {
        'x': nrm(ks[0], (BATCH, SEQ, D_MODEL), 1.0),
        'p': nrm(ks[1], (DEPTH, BATCH, SEQ, PLE_DIM), 1.0),
        'w_in': jnp.concatenate([w_qk, w_v, w_pool_in, w_gate], axis=-1),
        'b_gate': nrm(ks[6], (L, N_BRANCHES * D_MODEL), 0.02),
        'lambda_q1': nrm(ks[7], (L, DIFF_HEAD_DIM), 0.1),
        'lambda_k1': nrm(ks[8], (L, DIFF_HEAD_DIM), 0.1),
        'lambda_q2': nrm(ks[9], (L, DIFF_HEAD_DIM), 0.1),
        'lambda_k2': nrm(ks[10], (L, DIFF_HEAD_DIM), 0.1),
        'subln_g': 1.0 + nrm(ks[11], (L, 2 * DIFF_HEAD_DIM), 0.02),
        'w_attn_br': nrm(ks[12], (L, ATTN_WIDTH, D_MODEL), ATTN_WIDTH ** -0.5),
        'w_pool_mix': nrm(ks[13], (L, N_POOL_GROUPS, POOL_GROUP_DIM, POOL_GROUP_DIM), POOL_GROUP_DIM ** -0.5),
        'pool_scale': 1.0 + nrm(ks[14], (L, POOL_WIDTH), 0.02),
        'w_pool_br': nrm(ks[15], (L, POOL_WIDTH, D_MODEL), POOL_WIDTH ** -0.5),
        'w_out': nrm(ks[16], (L, D_MODEL, D_MODEL), s_in * DEEPNORM_BETA),
        'ln1_g': 1.0 + nrm(ks[17], (L, D_MODEL), 0.02),
        'ln1_b': nrm(ks[18], (L, D_MODEL), 0.02),
        'w_router': nrm(ks[19], (L, D_MODEL, N_EXPERTS), s_in),
        'b_router': nrm(ks[20], (L, N_EXPERTS), 0.01),
        'w_up': nrm(ks[21], (L, N_EXPERTS, D_MODEL, 2 * D_FF), s_in),
        'b_up': nrm(ks[22], (L, N_EXPERTS, 2 * D_FF), 0.02),
        'w_down': nrm(ks[23], (L, N_EXPERTS, D_FF, D_MODEL), D_FF ** -0.5 * DEEPNORM_BETA),
        'b_down': nrm(ks[24], (L, N_EXPERTS, D_MODEL), 0.02),
        'w_ple_gate': nrm(ks[25], (L, D_MODEL, D_MODEL), s_in),
        'w_ple_proj': nrm(ks[26], (L, PLE_DIM, D_MODEL), PLE_DIM ** -0.5 * DEEPNORM_BETA),
        'ln2_g': 1.0 + nrm(ks[27], (L, D_MODEL), 0.02),
        'ln2_b': nrm(ks[28], (L, D_MODEL), 0.02),
    }


def reference(x, p, w_in, b_gate, lambda_q1, lambda_k1, lambda_q2, lambda_k2, subln_g, w_attn_br,
              w_pool_mix, pool_scale, w_pool_br, w_out, ln1_g, ln1_b, w_router, b_router, w_up, b_up,
              w_down, b_down, w_ple_gate, w_ple_proj, ln2_g, ln2_b):
    Bb, Ss, _ = x.shape
    splits = [ATTN_WIDTH, 2 * ATTN_WIDTH, 3 * ATTN_WIDTH, 3 * ATTN_WIDTH + POOL_WIDTH]
    for i in range(DEPTH):
        proj = x @ w_in[i]
        q, k, v, u, g = jnp.split(proj, splits, axis=-1)
        q = q.reshape(Bb, Ss, N_DIFF_HEADS, 2, DIFF_HEAD_DIM)
        k = k.reshape(Bb, Ss, N_DIFF_HEADS, 2, DIFF_HEAD_DIM)
        v = v.reshape(Bb, Ss, N_DIFF_HEADS, 2 * DIFF_HEAD_DIM)
        lam_init = 0.8 - 0.6 * math.exp(-0.3 * i)
        lam = (jnp.exp(jnp.sum(lambda_q1[i].astype(F32) * lambda_k1[i].astype(F32)))
               - jnp.exp(jnp.sum(lambda_q2[i].astype(F32) * lambda_k2[i].astype(F32))) + lam_init)
        o = diff_attention(q, k, v, lam)
        o = rms_norm(o, subln_g[i]) * (1.0 - lam_init)
        a_branch = o.reshape(Bb, Ss, ATTN_WIDTH) @ w_attn_br[i]
        p_branch = multiscale_pool(u, w_pool_mix[i], pool_scale[i]) @ w_pool_br[i]
        g_a, g_p = jnp.split(jax.nn.sigmoid(g + b_gate[i]), 2, axis=-1)
        mixed = g_a * a_branch + g_p * p_branch
        x = layer_norm(DEEPNORM_ALPHA * x + mixed @ w_out[i], ln1_g[i], ln1_b[i])
        ffn = moe(x, w_router[i], b_router[i], w_up[i], b_up[i], w_down[i], b_down[i])
        ple = jax.nn.sigmoid(x @ w_ple_gate[i]) * (p[i] @ w_ple_proj[i])
        x = layer_norm(DEEPNORM_ALPHA * x + ffn + ple, ln2_g[i], ln2_b[i])
    return x
```

```python
import math
from contextlib import ExitStack
import numpy as np
import concourse.bass as bass
import concourse.mybir as mybir
from concourse.bass_utils import run_bass_kernel_spmd

F32 = mybir.dt.float32
BF16 = mybir.dt.bfloat16
I32 = mybir.dt.int32
AF = mybir.ActivationFunctionType
ALU = mybir.AluOpType

D = 1024
NCORES = 8
ALPHA = 2.0 ** 0.25
LAM_INIT = 0.2
EPS = 1e-5
NEG = -30000.0


class Queue:
    def __init__(self, name, sem, ring):
        self.name = name
        self.sem = sem
        self.count = 0
        self.seen = {}
        self.ops = []
        self.ring = ring
        self.ring_val = [0] * len(ring)
        self.dma_n = 0


class Sched:
    def __init__(self, nc):
        self.nc = nc
        self.q = {}
        self.writers = {}
        self.readers = {}
        self.sems = {}

    def add_queue(self, name, sem, ring=()):
        self.q[name] = Queue(name, sem, list(ring))
        if sem is not None:
            self.sems[id(sem)] = sem
        for s in ring:
            self.sems[id(s)] = s

    def _deps(self, q, r, w):
        deps = {}

        def add(tok):
            sid, val = tok
            if q.name == 'pe' and q.sem is not None and sid == id(q.sem):
                return
            if deps.get(sid, 0) < val:
                deps[sid] = val
        for k in r:
            for tok in self.writers.get(k, {}).items():
                add(tok)
        for k in w:
            for tok in self.writers.get(k, {}).items():
                add(tok)
            for tok in self.readers.get(k, {}).items():
                add(tok)
        waits = []
        for sid, val in deps.items():
            if q.seen.get(sid, 0) < val:
                q.seen[sid] = val
                waits.append((self.sems[sid], val))
        return waits

    def _commit(self, tok, r, w):
        sid, val = tok
        for k in r:
            d = self.readers.setdefault(k, {})
            if d.get(sid, 0) < val:
                d[sid] = val
        for k in w:
            self.writers[k] = {sid: val}
            self.readers[k] = {}

    def op(self, qn, fn, r=(), w=(), signal=True, extra_waits=()):
        q = self.q[qn]
        waits = self._deps(q, r, w)
        for tok in extra_waits:
            sid, val = tok
            if q.seen.get(sid, 0) < val:
                q.seen[sid] = val
                waits.append((self.sems[sid], val))
        if signal:
            q.count += 1
            tok = (id(q.sem), q.count)
            q.ops.append((waits, fn, (q.sem, 1)))
        else:
            tok = (id(q.sem), q.count + 1)
            q.ops.append((waits, fn, None))
        self._commit(tok, r, w)
        return tok

    def dma(self, qn, fn, r=(), w=()):
        q = self.q[qn]
        slot = q.dma_n % len(q.ring)
        q.dma_n += 1
        sem = q.ring[slot]
        waits = self._deps(q, r, w)
        prev = q.ring_val[slot]
        if prev > 0 and q.seen.get(id(sem), 0) < prev:
            q.seen[id(sem)] = prev
            waits.append((sem, prev))
        q.ring_val[slot] = prev + 16
        tok = (id(sem), prev + 16)
        q.ops.append((waits, fn, (sem, 16)))
        self._commit(tok, r, w)
        return tok

    def wait_keys(self, qn, keys):
        q = self.q[qn]
        waits = self._deps(q, (), keys)
        q.ops.append((waits, None, None))

    def barrier(self):
        toks = []
        for q in self.q.values():
            if q.sem is not None and q.count > 0:
                toks.append((id(q.sem), q.count))
            for s, v in zip(q.ring, q.ring_val):
                if v > 0:
                    toks.append((id(s), v))
        for q in self.q.values():
            waits = []
            for sid, val in toks:
                if q.name == 'pe' and q.sem is not None and sid == id(q.sem):
                    continue
                if q.seen.get(sid, 0) < val:
                    q.seen[sid] = val
                    waits.append((self.sems[sid], val))
            q.ops.append((waits, None, None))

    def replay(self, qn, eng):
        for waits, fn, inc in self.q[qn].ops:
            for sem, val in waits:
                eng.wait_ge(sem, val)
            if fn is not None:
                ins = fn(eng)
                if inc is not None:
                    ins.then_inc(inc[0], inc[1])


def build(cfg):
    S = cfg['S']
    NSEQ = cfg['NSEQ']
    C = cfg['C']
    NE = cfg.get('NE', 32)
    phases = cfg.get('phases', 'ABCD')
    dbg = cfg.get('dbg', False)
    T = S * NSEQ
    NCH = S // 512
    NKT = S // 128
    NT = T // 128
    CB = C // 128
    NSLOT = NE * C

    nc = bass.Bass("TRN2", target_bir_lowering=False)

    def din(name, shape, dt=F32):
        return nc.dram_tensor(name, list(shape), dt, kind="ExternalInput").ap()

    x_d = din("x", [T, D])
    p_d = din("p", [T, 256])
    w_in_d = din("w_in", [D, 4096])
    b_gate_d = din("b_gate", [128, 16])
    lam_d = din("lam", [1, 256])
    subln_d = din("subln_g", [1, 128])
    w_ab_d = din("w_attn_br", [512, D])
    w_mix_d = din("w_pool_mix", [4, 128, 128])
    pscale_d = din("pool_scale", [128, 4])
    w_pb_d = din("w_pool_br", [512, D])
    w_out_d = din("w_out", [D, D])
    ln1g_d = din("ln1_g", [1, D])
    ln1b_d = din("ln1_b", [1, D])
    w_r_d = din("w_router", [D, 32])
    b_r_d = din("b_router", [1, 32])
    w_up_d = din("w_up", [32, D, 2048])
    b_up_d = din("b_up", [128, 32 * 16])
    w_dn_d = din("w_down", [32, D, D])
    b_dn_d = din("b_down", [32, D])
    w_pg_d = din("w_ple_gate", [D, D])
    w_pp_d = din("w_ple_proj", [256, D])
    ln2g_d = din("ln2_g", [1, D])
    ln2b_d = din("ln2_b", [1, D])
    ident_d = din("c_ident", [128, 128])
    tri_d = din("c_tri", [128, 128])
    mask_d = din("c_mask", [128, 512])
    alibi_d = din("c_alibi", [128, 4 * NKT])
    ratio_d = din("c_ratio", [128, 64])
    carry0_d = din("c_carry0", [128, 32])
    band_d = din("c_band", [128, 4 * 3 * 128])

    out_d = nc.dram_tensor("out", [T, D], F32, kind="ExternalOutput").ap()
    atd = nc.dram_tensor("atd", [4, 128, T], BF16, kind="Internal").ap()
    xs_d = nc.dram_tensor("xs", [NSLOT + 128, D], BF16, kind="Internal").ap()
    ys_d = nc.dram_tensor("ys", [NSLOT + 128, D], F32, kind="Internal").ap()
    base_d = nc.dram_tensor("base", [T, D], F32, kind="Internal").ap()
    dbg_d = None
    if dbg:
        dbg_d = nc.dram_tensor("dbg", [T, D], F32, kind="ExternalOutput").ap()

    es = ExitStack()
    with es:
        def sb(name, shape, dt, stack=es):
            return stack.enter_context(nc.sbuf_tensor(name, list(shape), dt))

        def ps(name, shape, dt, stack=es):
            return stack.enter_context(nc.psum_tensor(name, list(shape), dt))

        def sem(name):
            return es.enter_context(nc.semaphore(name))

        sc = Sched(nc)
        sc.add_queue('pe', sem('s_pe'))
        sc.add_queue('act', sem('s_act'))
        sc.add_queue('dve', sem('s_dve'))
        sc.add_queue('pool', sem('s_pool'), [sem('r_pool%d' % i) for i in range(8)])
        sc.add_queue('sp', None, [sem('r_sp%d' % i) for i in range(8)])

        def mm(out, lhsT, rhs, start, stop, r, w, signal):
            return sc.op('pe', lambda e: e.matmul(out, lhsT=lhsT, rhs=rhs, start=start, stop=stop,
                                                  skip_group_check=True), r=r, w=w, signal=signal)

        def tr(out, in_, ident, r, w, signal):
            return sc.op('pe', lambda e: e.transpose(out, in_, ident), r=r, w=w, signal=signal)

        def act(out, in_, func, r, w, bias=None, scale=None, accum_out=None):
            kw = {}
            if bias is not None:
                kw['bias'] = bias
            if scale is not None:
                kw['scale'] = scale
            if accum_out is not None:
                kw['accum_out'] = accum_out
            return sc.op('act', lambda e: e.activation(out, in_, func, **kw), r=r, w=w)

        def ts(qn, out, in0, s1, s2, op0, op1, r, w, accum_out=None):
            kw = {}
            if accum_out is not None:
                kw['accum_out'] = accum_out
            if op1 is None:
                return sc.op(qn, lambda e: e.tensor_scalar(out, in0, s1, None, op0, **kw), r=r, w=w)
            return sc.op(qn, lambda e: e.tensor_scalar(out, in0, s1, s2, op0, op1, **kw), r=r, w=w)

        def tt(qn, out, in0, in1, op, r, w):
            return sc.op(qn, lambda e: e.tensor_tensor(out, in0, in1, op), r=r, w=w)

        def stt(out, in0, scalar, in1, op0, op1, r, w, accum_out=None):
            kw = {}
            if accum_out is not None:
                kw['accum_out'] = accum_out
            return sc.op('dve', lambda e: e.scalar_tensor_tensor(out, in0, scalar, in1, op0, op1, **kw),
                         r=r, w=w)

        def cp(qn, out, in_, r, w):
            return sc.op(qn, lambda e: e.tensor_copy(out, in_), r=r, w=w)

        def memset(qn, ap, val, w):
            return sc.op(qn, lambda e: e.memset(ap, val), r=(), w=w)

        def dma(qn, out, in_, r, w):
            return sc.dma(qn, lambda e: e.dma_start(out=out, in_=in_), r=r, w=w)

        banks = [ps('bank%d' % i, [128, 512], F32) for i in range(8)]

        def bk(i):
            return ('ps', i)

        identF = sb('identF', [128, 128], F32)
        identB = sb('identB', [128, 128], BF16)
        dma('sp', identF[:], ident_d[:, :], (), ['identF'])
        dma('pool', identB[:], ident_d[:, :], (), ['identB'])

        if 'A' in phases:
            pa = ExitStack()
            with pa:
                Wqk = sb('Wqk', [128, 8, 1024], BF16, pa)
                Wv = sb('Wv', [128, 8, 512], BF16, pa)
                w_in_v = w_in_d.rearrange("(kc p) n -> p kc n", p=128)
                for kc in range(8):
                    dma('pool', Wqk[:, kc, :], w_in_v[:, kc, 0:1024], (), [('Wqk', kc)])
                    dma('pool', Wv[:, kc, :], w_in_v[:, kc, 1024:1536], (), [('Wv', kc)])
                wqk_keys = [('Wqk', kc) for kc in range(8)]
                wv_keys = [('Wv', kc) for kc in range(8)]
                maskB = sb('maskB', [128, 512], BF16, pa)
                dma('pool', maskB[:], mask_d[:, :], (), ['maskB'])
                alibi = sb('alibi', [128, 4 * NKT], F32, pa)
                dma('sp', alibi[:], alibi_d[:, :], (), ['alibi'])
                g2 = sb('g2', [128, 128], F32, pa)
                dma('sp', g2[:], subln_d.to_broadcast([128, 128]), (), ['g2'])
                lamt = sb('lamt', [128, 256], F32, pa)
                dma('sp', lamt[:], lam_d.to_broadcast([128, 256]), (), ['lamt'])
                lamw = sb('lamw', [128, 8], F32, pa)
                junk64 = sb('junk64', [128, 64], F32, pa)
                stt(junk64[:], lamt[:, 0:64], 1.0, lamt[:, 64:128], ALU.mult, ALU.mult, ['lamt'], ['junk64', 'lamw'],
                    accum_out=lamw[:, 0:1])
                stt(junk64[:], lamt[:, 128:192], 1.0, lamt[:, 192:256], ALU.mult, ALU.mult, ['lamt'], ['junk64', 'lamw'],
                    accum_out=lamw[:, 1:2])
                act(lamw[:, 2:4], lamw[:, 0:2], AF.Exp, ['lamw'], ['lamw'])
                stt(lamw[:, 4:5], lamw[:, 3:4], -LAM_INIT, lamw[:, 2:3], ALU.add, ALU.subtract, ['lamw'], ['lamw'])
                ts('dve', g2[:], g2[:], 1.0 - LAM_INIT, None, ALU.mult, None, ['g2'], ['g2'])

                KT = sb('KT', [128, 4, S], BF16, pa)
                Va = sb('Va', [128, NKT, 4, 129], BF16, pa)
                memset('pool', Va[:, :, :, 128:129], 1.0, ['Va1'])
                xin = [sb('xinA%d' % i, [128, 4, D], BF16, pa) for i in range(2)]
                XT = [sb('XTA%d' % i, [128, 8, 512], BF16, pa) for i in range(2)]
                mhalf = sb('mhalfA', [128, 4], F32, pa)
                memset('pool', mhalf[:], -0.5, ['mhalfA'])
                QT = [sb('QT%d' % i, [128, 4, 512], BF16, pa) for i in range(2)]
                NEB = 8
                Eb = [sb('Eb%d' % i, [128, 512], BF16, pa) for i in range(NEB)]
                Om = [sb('Om%d' % i, [128, 4, 128], F32, pa) for i in range(2)]
                rs = sb('rs', [128, 16], F32, pa)
                raw = sb('rawA', [128, 2, 2, 258], F32, pa)
                odiff = sb('odiff', [128, 4, 128], F32, pa)
                osq = sb('osq', [128, 128], F32, pa)
                onb = sb('onb', [128, 4, 128], BF16, pa)
                ATc = [sb('ATc%d' % i, [128, 4, 512], BF16, pa) for i in range(2)]
                ebn = [0]
                pend_epi = [None]
                pbn = [0]

                def pbank():
                    b = pbn[0] % 8
                    pbn[0] += 1
                    return b

                def sbank(m, dd):
                    return (2 + m) if dd % 2 == 0 else m

                def run_epi():
                    f = pend_epi[0]
                    pend_epi[0] = None
                    if f is not None:
                        f()

                for s in range(NSEQ):
                    for j in range(NCH):
                        ci = s * NCH + j
                        b2 = ci % 2
                        t0 = s * S + j * 512
                        dma('pool', xin[b2][:], x_d[t0:t0 + 512, :].rearrange("(tt p) d -> p tt d", p=128),
                            (), [('xinA', b2)])
                        for kc in range(8):
                            bnk = pbank()
                            tbk = banks[bnk][:].bitcast(BF16)
                            for tt_ in range(4):
                                tr(tbk[:, tt_ * 128:(tt_ + 1) * 128], xin[b2][:, tt_, kc * 128:(kc + 1) * 128],
                                   identB[:], [('xinA', b2), 'identB'], [bk(bnk)], signal=(tt_ == 3))
                            cp('dve', XT[b2][:, kc, :], tbk[:, 0:512], [], [bk(bnk), ('XTA', b2, kc)])
                        xt_keys = [('XTA', b2, kc) for kc in range(8)]
                        for c in range(8):
                            bnk = pbank()
                            for kc in range(8):
                                mm(banks[bnk][:], Wqk[:, kc, c * 128:(c + 1) * 128], XT[b2][:, kc, :],
                                   kc == 0, kc == 7, [('Wqk', kc), ('XTA', b2, kc)], [bk(bnk)], signal=(kc == 7))
                            if c < 4:
                                dst, dk = QT[b2][:, c, :], ('QT', b2, c)
                            else:
                                dst, dk = KT[:, c - 4, j * 512:(j + 1) * 512], ('KT', c - 4, j)
                            cp('dve', dst, banks[bnk][:], [], [bk(bnk), dk])
                        for tt_ in range(4):
                            bnk = pbank()
                            for kc in range(8):
                                mm(banks[bnk][:], XT[b2][:, kc, tt_ * 128:(tt_ + 1) * 128], Wv[:, kc, :],
                                   kc == 0, kc == 7, [('Wv', kc), ('XTA', b2, kc)], [bk(bnk)], signal=(kc == 7))
                            kt = j * 4 + tt_
                            dst = Va[:, kt, :, 0:128]
                            src = banks[bnk][:].rearrange("p (h d) -> p h d", h=4)
                            cp('dve', dst, src, [], [bk(bnk), ('Va', kt)])
                        for h in range(4):
                            accb = [[banks[4 + 2 * m], banks[5 + 2 * m]] for m in range(2)]
                            acck = [[bk(4 + 2 * m), bk(5 + 2 * m)] for m in range(2)]
                            ndd = min(4 * j + 4, (6, 18, 1 << 30, 1 << 30)[h])
                            pendq = []

                            def pv(dd, i0, ebs):
                                for m in range(2):
                                    for i in range(i0, 4):
                                        kt = 4 * j + i - dd
                                        a = accb[m][i // 2][:, (i % 2) * 129:(i % 2) * 129 + 129]
                                        mm(a, Eb[ebs[m]][:, i * 128:(i + 1) * 128], Va[:, kt, h, :],
                                           (dd == 0 and i % 2 == 0), False,
                                           [('Eb', ebs[m]), ('Va', kt), 'Va1'], [acck[m][i // 2]],
                                           signal=(i == 3))

                            for dd in range(ndd):
                                i0 = max(0, dd - 4 * j)
                                for i in range(i0, 4):
                                    kt = 4 * j + i - dd
                                    last = (i == 3) and dd != 0
                                    for m in range(2):
                                        pb = m * 64
                                        sbk = sbank(m, dd)
                                        mm(banks[sbk][:, i * 128:(i + 1) * 128],
                                           KT[pb:pb + 64, h, kt * 128:(kt + 1) * 128],
                                           QT[b2][pb:pb + 64, h, i * 128:(i + 1) * 128],
                                           i == i0, False, [('KT', h, kt // 4), ('QT', b2, h)], [bk(sbk)],
                                           signal=last)
                                if dd == 0:
                                    for m in range(2):
                                        sbk = sbank(m, dd)
                                        mm(banks[sbk][:], identB[:], maskB[:], False, True,
                                           ['identB', 'maskB'], [bk(sbk)], signal=True)
                                bcol = h * NKT + dd
                                ebs = []
                                for m in range(2):
                                    eb = ebn[0] % NEB
                                    ebn[0] += 1
                                    ebs.append(eb)
                                    sbk = sbank(m, dd)
                                    act(Eb[eb][:, i0 * 128:512], banks[sbk][:, i0 * 128:512], AF.Exp,
                                        ['alibi'], [bk(sbk), ('Eb', eb)],
                                        bias=alibi[:, bcol:bcol + 1], scale=0.125)
                                pendq.append((dd, i0, ebs))
                                if len(pendq) > 2:
                                    pv(*pendq.pop(0))
                                if dd == min(9, ndd - 1):
                                    run_epi()
                            while pendq:
                                pv(*pendq.pop(0))
                            for m in range(2):
                                for hb_ in range(2):
                                    rk = ('raw', m, hb_)
                                    cp('dve', raw[:, m, hb_, :], accb[m][hb_][:, 0:258], [], [acck[m][hb_], rk])
                            for m in range(2):
                                for i in range(4):
                                    o = (i % 2) * 129
                                    col = m * 4 + i
                                    rk = ('raw', m, i // 2)
                                    sc.op('dve', (lambda e, m=m, i=i, o=o, col=col: e.reciprocal(
                                        rs[:, col:col + 1], raw[:, m, i // 2, o + 128:o + 129])),
                                          r=[rk], w=[('rs', col)])
                                    ts('dve', Om[m][:, i, :], raw[:, m, i // 2, o:o + 128], rs[:, col:col + 1], None,
                                       ALU.mult, None, [rk, ('rs', col)], [('Om', m, i)])
                            def make_epi(h=h, b2=b2, ci=ci, t0=t0):
                                def epi():
                                    for i in range(4):
                                        stt(odiff[:, i, :], Om[1][:, i, :], lamw[:, 4:5], Om[0][:, i, :], ALU.mult, ALU.add,
                                            ['lamw', ('Om', 0, i), ('Om', 1, i)], [('odiff', i)])
                                        stt(osq[:], odiff[:, i, :], 1.0, odiff[:, i, :], ALU.mult, ALU.mult,
                                            [('odiff', i)], ['osq', ('rs', 8 + i)], accum_out=rs[:, 8 + i:9 + i])
                                    ts('pool', rs[:, 8:12], rs[:, 8:12], 1.0 / 128.0, EPS, ALU.mult, ALU.add,
                                       [('rs', 8 + i) for i in range(4)], [('rs', 8 + i) for i in range(4)])
                                    tt('pool', rs[:, 12:16], rs[:, 8:12], mhalf[:], ALU.pow,
                                       [('rs', 8 + i) for i in range(4)] + ['mhalfA'], [('rs', 12 + i) for i in range(4)])
                                    for i in range(4):
                                        stt(onb[:, i, :], odiff[:, i, :], rs[:, 12 + i:13 + i], g2[:], ALU.mult, ALU.mult,
                                            [('odiff', i), ('rs', 12 + i), 'g2'], [('onb', i)])
                                    tb = h % 2
                                    tbank = banks[tb][:].bitcast(BF16)
                                    for i in range(4):
                                        tr(tbank[:, i * 128:(i + 1) * 128], onb[:, i, :], identB[:],
                                           [('onb', i), 'identB'], [bk(tb)], signal=(i == 3))
                                    cp('dve', ATc[b2][:, h, :], tbank[:, 0:512], [], [bk(tb), ('ATc', b2, h)])
                                    if h == 3:
                                        dma('sp', atd[:, :, t0:t0 + 512].rearrange("h p t -> p h t"), ATc[b2][:],
                                            [('ATc', b2, hh) for hh in range(4)], [('atd', ci)])
                                return epi
                            pend_epi[0] = make_epi()
                run_epi()
            sc.barrier()

        if dbg == 'A':
            with ExitStack() as pd:
                tmpb = sb('dbgb', [128, 4, T], BF16, pd)
                tmpf = sb('dbgf', [128, 4, T], F32, pd)
                dma('sp', tmpb[:], atd.rearrange("h p t -> p h t"), [('atd', ci) for ci in range(NSEQ * NCH)], ['dbgb'])
                cp('dve', tmpf[:], tmpb[:], ['dbgb'], ['dbgf'])
                dma('sp', dbg_d[0:512, 0:T].rearrange("(h p) t -> p h t", p=128), tmpf[:], ['dbgf'], ['dbgout'])
                sc.wait_keys('sp', ['dbgout'])
                sc.barrier()


        slot4 = sb('slot4', [128, NT, 4], I32)
        gate4 = sb('gate4', [128, NT, 4], F32)

        if 'B' in phases:
            pbs = ExitStack()
            with pbs:
                w_in_v = w_in_d.rearrange("(kc p) n -> p kc n", p=128)
                Wu = sb('Wu', [128, 8, 512], BF16, pbs)
                Wg = sb('Wg', [128, 8, 2048], BF16, pbs)
                Wab = sb('Wab', [128, 4, D], BF16, pbs)
                Wpb = sb('Wpb', [128, 4, D], BF16, pbs)
                Wmix = sb('Wmix', [128, 4, 128], BF16, pbs)
                Wout = sb('Wout', [128, 8, D], BF16, pbs)
                Wpg = sb('Wpg', [128, 8, D], BF16, pbs)
                Wpp = sb('Wpp', [128, 2, D], BF16, pbs)
                Wr = sb('Wr', [128, 8, 32], F32, pbs)
                for kc in range(8):
                    dma('pool', Wu[:, kc, :], w_in_v[:, kc, 1536:2048], (), [('Wu', kc)])
                for h in range(4):
                    dma('pool', Wmix[:, h, :], w_mix_d[h], (), [('Wmix', h)])
                for kc in range(8):
                    dma('pool', Wg[:, kc, :], w_in_v[:, kc, 2048:4096], (), [('Wg', kc)])
                for h in range(4):
                    dma('pool', Wab[:, h, :], w_ab_d[h * 128:(h + 1) * 128, :], (), [('Wab', h)])
                    dma('pool', Wpb[:, h, :], w_pb_d[h * 128:(h + 1) * 128, :], (), [('Wpb', h)])
                for kc in range(8):
                    dma('pool', Wout[:, kc, :], w_out_d[kc * 128:(kc + 1) * 128, :], (), [('Wout', kc)])
                for kc in range(8):
                    dma('pool', Wpg[:, kc, :], w_pg_d[kc * 128:(kc + 1) * 128, :], (), [('Wpg', kc)])
                for c2 in range(2):
                    dma('pool', Wpp[:, c2, :], w_pp_d[c2 * 128:(c2 + 1) * 128, :], (), [('Wpp', c2)])
                dma('sp', Wr[:], w_r_d.rearrange("(kc p) n -> p kc n", p=128), (), ['Wr'])
                bgate = sb('bgate', [128, 16], F32, pbs)
                dma('sp', bgate[:], b_gate_d[:, :], (), ['bgate'])
                pscale = sb('pscale', [128, 4], F32, pbs)
                dma('sp', pscale[:], pscale_d[:, :], (), ['pscale'])
                ln1g = sb('ln1g', [128, D], F32, pbs)
                ln1b = sb('ln1b', [128, D], F32, pbs)
                dma('sp', ln1g[:], ln1g_d.to_broadcast([128, D]), (), ['ln1g'])
                dma('sp', ln1b[:], ln1b_d.to_broadcast([128, D]), (), ['ln1b'])
                brt = sb('brt', [128, 32], F32, pbs)
                dma('sp', brt[:], b_r_d.to_broadcast([128, 32]), (), ['brt'])
                triB = sb('triB', [128, 128], BF16, pbs)
                dma('pool', triB[:], tri_d[:, :], (), ['triB'])
                onesB = sb('onesB', [128, 128], BF16, pbs)
                memset('pool', onesB[:], 1.0, ['onesB'])
                carry = sb('carry', [128, 32], F32, pbs)
                dma('sp', carry[:], carry0_d[:, :], (), ['carry'])

                Bc = sb('Bc', [128, 4, 3, 128], BF16, pbs)
                dma('pool', Bc[:].rearrange("p g k t -> p (g k t)"), band_d[:, :], (), ['Bc'])
                xin = [sb('xinB%d' % i, [128, D], F32, pbs) for i in range(2)]
                xbb = [sb('xbB%d' % i, [128, D], BF16, pbs) for i in range(2)]
                mhalfB = sb('mhalfB', [128, 4], F32, pbs)
                memset('pool', mhalfB[:], -0.5, ['mhalfB'])
                eB = sb('eB', [128, 4], F32, pbs)
                memset('pool', eB[:], math.e, ['eB'])
                XT = sb('XTB', [128, 8, 512], BF16, pbs)
                ATl = sb('ATl', [128, 4, 512], BF16, pbs)
                pin = sb('pin', [128, 4, 256], BF16, pbs)
                PinT = [sb('PinT%d' % i, [128, 2, 512], BF16, pbs) for i in range(2)]
                U = sb('U', [128, 5, 512], BF16, pbs)
                PT = sb('PT', [128, 4, 512], BF16, pbs)
                MT = sb('MT', [128, 4, 512], BF16, pbs)
                sag = [[sb('sag%d%d' % (i, k), [128, 512], F32, pbs) for k in range(2)] for i in range(2)]
                mixT = [sb('mixT%d' % i, [128, 8, 512], BF16, pbs) for i in range(2)]
                Y = [sb('Y%d' % i, [128, D], F32, pbs) for i in range(3)]
                X1 = [sb('X1_%d' % i, [128, D], F32, pbs) for i in range(2)]
                x1b = [sb('x1b%d' % i, [128, D], BF16, pbs) for i in range(2)]
                X1T32 = sb('X1T32', [128, 8, 128], F32, pbs)
                X1Tb = sb('X1Tb', [128, 8, 128], BF16, pbs)
                stB = sb('stB', [128, 2, 12], F32, pbs)
                smB = sb('smB', [128, 2, 16], F32, pbs)
                Lr = sb('Lr', [128, 32], F32, pbs)
                V8 = sb('V8', [128, 8], F32, pbs)
                Mb = sb('Mb', [128, 32], BF16, pbs)
                slotf = sb('slotf', [128, 32], F32, pbs)
                junk32 = sb('junk32', [128, 32], F32, pbs)
                s4f = sb('s4f', [128, 4], F32, pbs)
                e4 = sb('e4', [128, 4], F32, pbs)
                nbk = [0]

                held = set()

                def nb():
                    while True:
                        b = nbk[0] % 8
                        nbk[0] += 1
                        if b not in held:
                            return b

                NCI = NSEQ * NCH
                xn = [0]

                def y_load(gt_):
                    dma('sp', Y[gt_ % 3][:], x_d[gt_ * 128:(gt_ + 1) * 128, :], (),
                        [('Y', gt_ % 3, 0), ('Y', gt_ % 3, 1)])

                def front_units(ci):
                    s, j = divmod(ci, NCH)
                    t0 = s * S + j * 512
                    mp = ci % 2
                    units = []

                    def u_x(tt_):
                        def f():
                            if tt_ == 0:
                                dma('sp', ATl[:], atd[:, :, t0:t0 + 512].rearrange("h p t -> p h t"), [('atd', ci)], ['ATl'])
                                dma('pool', pin[:], p_d[t0:t0 + 512, :].rearrange("(tt p) d -> p tt d", p=128), (), ['pin'])
                            xi = xn[0] % 2
                            xn[0] += 1
                            r0 = t0 + tt_ * 128
                            dma('sp', xin[xi][:], x_d[r0:r0 + 128, :], (), [('xinB', xi)])
                            act(xbb[xi][:], xin[xi][:], AF.Copy, [('xinB', xi)], [('xbB', xi)])
                            b = nb()
                            tbk = banks[b][:].bitcast(BF16)
                            for kc in range(8):
                                tr(tbk[:, kc * 128:(kc + 1) * 128], xbb[xi][:, kc * 128:(kc + 1) * 128],
                                   identB[:], [('xbB', xi), 'identB'], [bk(b)], signal=(kc == 7))
                            srcv = tbk[:, 0:1024].rearrange("p (k t) -> p k t", k=8)
                            if tt_ % 2 == 0:
                                cp('dve', XT[:, :, tt_ * 128:(tt_ + 1) * 128], srcv, [], [bk(b), ('XTB', tt_)])
                            else:
                                act(XT[:, :, tt_ * 128:(tt_ + 1) * 128], srcv, AF.Copy, [], [bk(b), ('XTB', tt_)])
                        return f

                    def u_u(tt_):
                        def f():
                            b = nb()
                            for kc in range(8):
                                mm(banks[b][:], XT[:, kc, tt_ * 128:(tt_ + 1) * 128], Wu[:, kc, :], kc == 0, kc == 7,
                                   [('Wu', kc), ('XTB', tt_)], [bk(b)], signal=(kc == 7))
                            if tt_ % 2 == 0:
                                act(U[:, 1 + tt_, :], banks[b][:], AF.Copy, [], [bk(b), ('U', 1 + tt_)])
                            else:
                                cp('dve', U[:, 1 + tt_, :], banks[b][:], [], [bk(b), ('U', 1 + tt_)])
                        return f

                    def u_p():
                        for c2 in range(2):
                            b = nb()
                            tbk = banks[b][:].bitcast(BF16)
                            for tt_ in range(4):
                                tr(tbk[:, tt_ * 128:(tt_ + 1) * 128], pin[:, tt_, c2 * 128:(c2 + 1) * 128],
                                   identB[:], ['pin', 'identB'], [bk(b)], signal=(tt_ == 3))
                            cp('dve', PinT[mp][:, c2, :], tbk[:, 0:512], [], [bk(b), ('PinT', mp, c2)])

                    def u_pool():
                        for g in range(4):
                            b = nb()
                            for tt_ in range(4):
                                o_ = banks[b][:, tt_ * 128:(tt_ + 1) * 128]
                                if j == 0 and tt_ == 0:
                                    mm(o_, U[:, 1, g * 128:(g + 1) * 128], Bc[:, g, 2, :], True, True,
                                       [('U', 1), 'Bc'], [bk(b)], signal=False)
                                else:
                                    mm(o_, U[:, 1 + tt_, g * 128:(g + 1) * 128], Bc[:, g, 0, :], True, False,
                                       [('U', 1 + tt_), 'Bc'], [bk(b)], signal=False)
                                    mm(o_, U[:, tt_, g * 128:(g + 1) * 128], Bc[:, g, 1, :], False, True,
                                       [('U', tt_), 'Bc'], [bk(b)], signal=(tt_ == 3))
                            if g % 2 == 0:
                                act(PT[:, g, :], banks[b][:], AF.Copy, [], [bk(b), ('PT', g)])
                            else:
                                cp('dve', PT[:, g, :], banks[b][:], [], [bk(b), ('PT', g)])
                        act(U[:, 0, :], U[:, 4, :], AF.Copy, [('U', 4)], [('U', 0)])
                        for g in range(4):
                            b = nb()
                            mm(banks[b][:], Wmix[:, g, :], PT[:, g, :], True, True, [('Wmix', g), ('PT', g)], [bk(b)],
                               signal=True)
                            act(MT[:, g, :], banks[b][:], AF.Copy, ['pscale'], [bk(b), ('MT', g)],
                                scale=pscale[:, g:g + 1])

                    def u_gate(c):
                        def f():
                            bGa, bGp, bA, bP = nb(), nb(), nb(), nb()
                            xtk = [('XTB', q_) for q_ in range(4)]
                            for kc in range(8):
                                mm(banks[bGa][:], Wg[:, kc, c * 128:(c + 1) * 128], XT[:, kc, :], kc == 0, kc == 7,
                                   [('Wg', kc)] + xtk, [bk(bGa)], signal=(kc == 7))
                            for kc in range(8):
                                mm(banks[bGp][:], Wg[:, kc, 1024 + c * 128:1024 + (c + 1) * 128], XT[:, kc, :],
                                   kc == 0, kc == 7, [('Wg', kc)] + xtk, [bk(bGp)], signal=(kc == 7))
                            for h in range(4):
                                mm(banks[bA][:], Wab[:, h, c * 128:(c + 1) * 128], ATl[:, h, :], h == 0, h == 3,
                                   [('Wab', h), 'ATl'], [bk(bA)], signal=(h == 3))
                            for g in range(4):
                                mm(banks[bP][:], Wpb[:, g, c * 128:(c + 1) * 128], MT[:, g, :], g == 0, g == 3,
                                   [('Wpb', g), ('MT', g)], [bk(bP)], signal=(g == 3))
                            sa, sp_ = sag[c % 2]
                            ka, kp_ = ('sag', c % 2, 0), ('sag', c % 2, 1)
                            act(sa[:], banks[bGa][:], AF.Sigmoid, ['bgate'], [bk(bGa), ka], bias=bgate[:, c:c + 1])
                            act(sp_[:], banks[bGp][:], AF.Sigmoid, ['bgate'], [bk(bGp), kp_],
                                bias=bgate[:, 8 + c:9 + c])
                            tt('dve', sa[:], sa[:], banks[bA][:], ALU.mult, [], [ka, bk(bA)])
                            tt('dve', sp_[:], sp_[:], banks[bP][:], ALU.mult, [], [kp_, bk(bP)])
                            tt('dve', mixT[mp][:, c, :], sa[:], sp_[:], ALU.add, [ka, kp_], [('mixT', mp, c)])
                        return f

                    units += [u_x(tt_) for tt_ in range(4)]
                    units += [u_u(tt_) for tt_ in range(4)]
                    units += [u_p, u_pool]
                    units += [u_gate(c) for c in range(8)]
                    return units

                def tail_units(ci):
                    s, j = divmod(ci, NCH)
                    t0 = s * S + j * 512
                    mp = ci % 2

                    def W_a(tt_, ci=ci, t0=t0):
                        pb_ = tt_ % 2
                        yb_ = ((t0 // 128) + tt_) % 3
                        r0 = t0 + tt_ * 128
                        gt_ = (t0 // 128) + tt_
                        if gt_ == 0:
                            y_load(0)
                        if gt_ + 1 < NT:
                            y_load(gt_ + 1)
                        b0, b1 = nb(), nb()
                        for half, b in ((0, b0), (1, b1)):
                            for kc in range(8):
                                mm(banks[b][:], mixT[mp][:, kc, tt_ * 128:(tt_ + 1) * 128],
                                   Wout[:, kc, half * 512:(half + 1) * 512], kc == 0, kc == 7,
                                   [('mixT', mp, kc), ('Wout', kc)], [bk(b)], signal=(kc == 7))
                            hs = slice(half * 512, (half + 1) * 512)
                            stt(Y[yb_][:, hs], Y[yb_][:, hs], ALPHA, banks[b][:], ALU.mult, ALU.add, [],
                                [bk(b), ('Y', yb_, half)])

                    def W_b(tt_, ci=ci, t0=t0):
                        pb_ = tt_ % 2
                        yb_ = ((t0 // 128) + tt_) % 3
                        src, dst = Y[yb_], X1[pb_]
                        yk = [('Y', yb_, 0), ('Y', yb_, 1)]
                        sc.op('dve', (lambda e, src=src, pb_=pb_: e.bn_stats(stB[:, pb_, 0:6], src[:, 0:512])),
                              r=[yk[0]], w=[('stB0', pb_)])
                        sc.op('dve', (lambda e, src=src, pb_=pb_: e.bn_stats(stB[:, pb_, 6:12], src[:, 512:1024])),
                              r=[yk[1]], w=[('stB1', pb_)])
                        sc.op('dve', (lambda e, pb_=pb_: e.bn_aggr(smB[:, pb_, 0:2], stB[:, pb_, 0:12])),
                              r=[('stB0', pb_), ('stB1', pb_)], w=[('smB', pb_)])
                        ts('dve', smB[:, pb_, 2:3], smB[:, pb_, 1:2], 1.0, EPS, ALU.mult, ALU.add, [('smB', pb_)],
                           [('smB2', pb_)])
                        tt('pool', smB[:, pb_, 3:4], smB[:, pb_, 2:3], mhalfB[:, 0:1], ALU.pow,
                           [('smB2', pb_), 'mhalfB'], [('smB3', pb_)])
                        ts('dve', dst[:], src[:], smB[:, pb_, 0:1], smB[:, pb_, 3:4], ALU.subtract, ALU.mult,
                           yk + [('smB', pb_), ('smB3', pb_)], [('X1', pb_)])
                        tt('dve', dst[:], dst[:], ln1g[:], ALU.mult, ['ln1g'], [('X1', pb_)])
                        tt('dve', dst[:], dst[:], ln1b[:], ALU.add, ['ln1b'], [('X1', pb_)])
                        if dbg == 'B':
                            tile = (t0 // 128) + tt_
                            dma('sp', dbg_d[tile * 128:(tile + 1) * 128, :], dst[:], [('X1', pb_)], [('dbgout', tile)])

                    tl_state = {}

                    def Tl_a(tt_, t0=t0, ci=ci):
                        pb_ = tt_ % 2
                        tile = (t0 // 128) + tt_
                        X1c = X1[pb_]
                        kx = ('X1', pb_)
                        for hb in range(2):
                            b = nb()
                            for k4 in range(4):
                                kc = hb * 4 + k4
                                tr(banks[b][:, k4 * 128:(k4 + 1) * 128], X1c[:, kc * 128:(kc + 1) * 128], identF[:],
                                   [kx, 'identF'], [bk(b)], signal=(k4 == 3))
                            act(X1T32[:, hb * 4:(hb + 1) * 4, :],
                                banks[b][:].rearrange("p (k t) -> p k t", k=4), AF.Copy, [], [bk(b), ('X1T32', hb)])
                            cp('dve', X1Tb[:, hb * 4:(hb + 1) * 4, :],
                               banks[b][:].rearrange("p (k t) -> p k t", k=4), [], [bk(b), ('X1Tb', hb)])
                        bR = nb()
                        for kc in range(8):
                            mm(banks[bR][:, 0:32], X1T32[:, kc, :], Wr[:, kc, :], kc == 0, kc == 7,
                               [('X1T32', kc // 4), 'Wr'], [bk(bR)], signal=(kc == 7))
                        pbanks = []
                        for half in range(2):
                            bG, bPp = nb(), nb()
                            pbanks.append((bG, bPp))
                            for kc in range(8):
                                mm(banks[bG][:], X1Tb[:, kc, :], Wpg[:, kc, half * 512:(half + 1) * 512],
                                   kc == 0, kc == 7, [('X1Tb', kc // 4), ('Wpg', kc)], [bk(bG)], signal=(kc == 7))
                            for c2 in range(2):
                                mm(banks[bPp][:], PinT[ci % 2][:, c2, tt_ * 128:(tt_ + 1) * 128],
                                   Wpp[:, c2, half * 512:(half + 1) * 512], c2 == 0, c2 == 1,
                                   [('PinT', ci % 2, c2), ('Wpp', c2)], [bk(bPp)], signal=(c2 == 1))
                        yb_ = tile % 3
                        Yc = Y[yb_]
                        Ptmp = X1T32[:].rearrange("p k t -> p (k t)")
                        for half in range(2):
                            bG, bPp = pbanks[half]
                            hs = slice(half * 512, (half + 1) * 512)
                            act(Yc[:, hs], banks[bG][:], AF.Sigmoid, [], [bk(bG), ('Y', yb_, half)])
                            act(Ptmp[:, hs], banks[bPp][:], AF.Copy, [], [bk(bPp), ('X1T32', half)])
                        tt('dve', Lr[:], banks[bR][:, 0:32], brt[:], ALU.add, ['brt'], [bk(bR), 'Lr'])
                        sc.op('dve', lambda e: e.max(out=V8[:], in_=Lr[:]), r=['Lr'], w=['V8'])
                        ts('dve', Mb[:], Lr[:], V8[:, 3:4], None, ALU.is_ge, None, ['Lr', 'V8'], ['Mb'])
                        bK = nb()
                        mm(banks[bK][:, 0:32], triB[:], Mb[:], True, False, ['triB', 'Mb'], [bk(bK)], signal=False)
                        mm(banks[bK][:, 32:64], onesB[:], Mb[:], False, True, ['onesB', 'Mb'], [bk(bK)], signal=True)
                        tt('dve', slotf[:], banks[bK][:, 0:32], carry[:], ALU.add, ['carry'], [bk(bK), 'slotf'])
                        tt('dve', carry[:], carry[:], banks[bK][:, 32:64], ALU.add, [], [bk(bK), 'carry'])
                        for jj in range(4):
                            stt(junk32[:], Lr[:], V8[:, jj:jj + 1], slotf[:], ALU.is_equal, ALU.mult,
                                ['Lr', 'V8', 'slotf'], ['junk32', ('s4f', jj)], accum_out=s4f[:, jj:jj + 1])
                        cp('dve', slot4[:, tile, :], s4f[:], [('s4f', jj) for jj in range(4)], [('slot4', tile)])

                    def Tl_b(tt_, t0=t0):
                        pb_ = tt_ % 2
                        tile = (t0 // 128) + tt_
                        yb_ = tile % 3
                        X1c, Yc = X1[pb_], Y[yb_]
                        kx = ('X1', pb_)
                        ts('dve', e4[:], V8[:, 0:4], V8[:, 0:1], None, ALU.subtract, None, ['V8'], ['e4'])
                        tt('pool', e4[:], eB[:], e4[:], ALU.pow, ['eB'], ['e4'])
                        Ptmp = X1T32[:].rearrange("p k t -> p (k t)")
                        for half in range(2):
                            hs = slice(half * 512, (half + 1) * 512)
                            yk = ('Y', yb_, half)
                            tt('dve', Yc[:, hs], Yc[:, hs], Ptmp[:, hs], ALU.mult, [('X1T32', half)], [yk])
                            stt(Yc[:, hs], X1c[:, hs], ALPHA, Yc[:, hs], ALU.mult, ALU.add, [kx], [yk])
                        dma('sp', base_d[tile * 128:(tile + 1) * 128, :], Yc[:],
                            [('Y', yb_, 0), ('Y', yb_, 1)], [('base', tile)])
                        ts('dve', junk32[:, 0:4], e4[:], 1.0, 0.0, ALU.mult, ALU.add, ['e4'], ['junk32', ('smB5', pb_)],
                           accum_out=smB[:, pb_, 5:6])
                        sc.op('dve', (lambda e, pb_=pb_: e.reciprocal(smB[:, pb_, 6:7], smB[:, pb_, 5:6])),
                              r=[('smB5', pb_)], w=[('smB6', pb_)])
                        ts('dve', gate4[:, tile, :], e4[:], smB[:, pb_, 6:7], None, ALU.mult, None,
                           ['e4', ('smB6', pb_)], [('gate4', tile)])
                        act(x1b[pb_][:], X1c[:], AF.Copy, [kx], [('x1b', pb_)])
                        for jj in range(4):
                            sc.dma('pool', (lambda e, tile=tile, jj=jj, pb_=pb_: e.indirect_dma_start(
                                out=xs_d[:, :], out_offset=bass.IndirectOffsetOnAxis(slot4[:, tile, jj:jj + 1], 0),
                                in_=x1b[pb_][:], in_offset=None)), r=[('x1b', pb_), ('slot4', tile)],
                                w=[('xs', tile, jj)])


                    return [lambda: W_a(0), lambda: W_b(0), lambda: W_a(1), lambda: Tl_a(0), lambda: W_b(1),
                            lambda: Tl_b(0), lambda: W_a(2), lambda: Tl_a(1), lambda: W_b(2), lambda: Tl_b(1),
                            lambda: W_a(3), lambda: Tl_a(2), lambda: W_b(3), lambda: Tl_b(2), lambda: Tl_a(3),
                            lambda: Tl_b(3)]

                for u_ in front_units(0):
                    u_()
                for ci in range(NCI):
                    tu = tail_units(ci)
                    fu = front_units(ci + 1) if ci + 1 < NCI else []
                    nt, nf = len(tu), len(fu)
                    fi = 0
                    for k_, t_ in enumerate(tu):
                        t_()
                        want = ((k_ + 1) * nf) // nt
                        while fi < want:
                            fu[fi]()
                            fi += 1
                if dbg == 'cnt':
                    dma('sp', dbg_d[0:128, 0:32], carry[:], ['carry'], ['dbgcnt'])
                    sc.wait_keys('sp', ['dbgcnt'])
            sc.barrier()

        if 'C' in phases:
            pcs = ExitStack()
            with pcs:
                Wup = [sb('Wup%d' % i, [128, 8, 2048], BF16, pcs) for i in range(2)]
                Wdn = [sb('Wdn%d' % i, [128, 8, D], BF16, pcs) for i in range(2)]
                bdn = [sb('bdn%d' % i, [128, D], F32, pcs) for i in range(2)]
                bup = sb('bup', [128, 512], F32, pcs)
                dma('sp', bup[:], b_up_d[:, :], (), ['bup'])
                XsT = [sb('XsT%d' % i, [128, 8, C], BF16, pcs) for i in range(2)]
                hT = sb('hT', [128, 8, C], BF16, pcs)
                yst = [sb('yst%d' % i, [128, D], F32, pcs) for i in range(2)]
                NSW = 3
                sw = [[sb('sw%d_%d' % (i, k), [128, 512], F32, pcs) for k in range(3)] for i in range(NSW)]
                swn = [0]
                nbk = [0]

                def nb():
                    b = nbk[0] % 8
                    nbk[0] += 1
                    return b
                all_xs = [('xs', t, jj) for t in range(NT) for jj in range(4)]

                def load_w(e):
                    pe_ = e % 2
                    wu = w_up_d[e].rearrange("(kc p) n -> p kc n", p=128)
                    wd = w_dn_d[e].rearrange("(kc p) n -> p kc n", p=128)
                    if e == 0:
                        for fb in range(8):
                            for off_, kk_ in ((0, fb), (1024, 8 + fb)):
                                c0 = off_ + fb * 128
                                dma('pool', Wup[0][:, :, c0:c0 + 128], wu[:, :, c0:c0 + 128], (), [('Wup0f', kk_)])
                    else:
                        extra = [('Wup0f', k_) for k_ in range(16)] if e == 2 else []
                        for kc in range(8):
                            dma('pool', Wup[pe_][:, kc, :], wu[:, kc, :], (), [('Wup', pe_, kc)] + extra)
                    for kc in range(8):
                        dma('pool', Wdn[pe_][:, kc, :], wd[:, kc, :], (), [('Wdn', pe_, kc)])
                    dma('pool', bdn[pe_][:], b_dn_d[e:e + 1, :].to_broadcast([128, D]), (), [('bdn', pe_)])

                def xs_tload(e):
                    for kc in range(8):
                        sc.dma('sp', (lambda q, e=e, kc=kc: q.dma_start_transpose(
                            out=XsT[e % 2][:, kc, :], in_=xs_d[e * C:(e + 1) * C, kc * 128:(kc + 1) * 128])),
                            r=(all_xs if e == 0 else []), w=[('XsT', e % 2, kc)])

                slot_tiles = []
                o = 0
                while o < C:
                    n = min(512, C - o)
                    slot_tiles.append((o, n))
                    o += n

                def up_unit(e, o, n, f):
                    pe_ = e % 2
                    bG, bL = nb(), nb()
                    for kc in range(8):
                        wk_ = ('Wup0f', f) if e == 0 else ('Wup', pe_, kc)
                        mm(banks[bG][:, 0:n], Wup[pe_][:, kc, f * 128:(f + 1) * 128], XsT[pe_][:, kc, o:o + n],
                           kc == 0, kc == 7, [wk_, ('XsT', pe_, kc)], [bk(bG)], signal=(kc == 7))
                    for kc in range(8):
                        wk_ = ('Wup0f', 8 + f) if e == 0 else ('Wup', pe_, kc)
                        mm(banks[bL][:, 0:n], Wup[pe_][:, kc, 1024 + f * 128:1024 + (f + 1) * 128],
                           XsT[pe_][:, kc, o:o + n], kc == 0, kc == 7, [wk_, ('XsT', pe_, kc)],
                           [bk(bL)], signal=(kc == 7))
                    si = swn[0] % NSW
                    swn[0] += 1
                    g1, sgt, l0 = sw[si]
                    kg1, ksg, kl0 = [('sw', si, k) for k in range(3)]
                    cg = e * 16 + f
                    cl = e * 16 + 8 + f
                    ts('dve', g1[:, 0:n], banks[bG][:, 0:n], bup[:, cg:cg + 1], 7.0, ALU.add, ALU.min,
                       ['bup'], [bk(bG), kg1])
                    act(sgt[:, 0:n], g1[:, 0:n], AF.Sigmoid, [kg1], [ksg], scale=1.702)
                    act(l0[:, 0:n], banks[bL][:, 0:n], AF.Identity, ['bup'], [bk(bL), kl0],
                        bias=bup[:, cl:cl + 1])
                    ts('dve', l0[:, 0:n], l0[:, 0:n], 7.0, -7.0, ALU.min, ALU.max, [], [kl0])
                    tt('dve', sgt[:, 0:n], g1[:, 0:n], sgt[:, 0:n], ALU.mult, [kg1], [ksg])
                    hks = [('hT', kb) for kb in range(o // 128, (o + n) // 128)]
                    stt(hT[:, f, o:o + n], l0[:, 0:n], 1.0, sgt[:, 0:n], ALU.add, ALU.mult, [kl0, ksg], hks)

                def down_blocks(e, o, n):
                    pe_ = e % 2
                    for sbk in range(o // 128, (o + n) // 128):
                        yb = yst[sbk % 2]
                        for half in range(2):
                            b = nb()
                            for fc in range(8):
                                mm(banks[b][:], hT[:, fc, sbk * 128:(sbk + 1) * 128],
                                   Wdn[pe_][:, fc, half * 512:(half + 1) * 512], fc == 0, fc == 7,
                                   [('hT', sbk), ('Wdn', pe_, fc)], [bk(b)], signal=(fc == 7))
                            hs = slice(half * 512, (half + 1) * 512)
                            tt('dve', yb[:, hs], banks[b][:], bdn[pe_][:, hs], ALU.add, [('bdn', pe_)],
                               [bk(b), ('yst', sbk % 2, half)])
                        r0 = e * C + sbk * 128
                        dma('sp', ys_d[r0:r0 + 128, :], yb[:], [('yst', sbk % 2, 0), ('yst', sbk % 2, 1)],
                            [('ys', e, sbk)])

                load_w(0)
                xs_tload(0)
                for e in range(NE):
                    if e + 1 < NE:
                        load_w(e + 1)
                        xs_tload(e + 1)
                    for si_, (o, n) in enumerate(slot_tiles):
                        for f in range(8):
                            up_unit(e, o, n, f)
                        if si_ >= 1:
                            down_blocks(e, *slot_tiles[si_ - 1])
                    down_blocks(e, *slot_tiles[-1])
            sc.barrier()

        if 'D' in phases:
            pds = ExitStack()
            with pds:
                ln2g = sb('ln2g', [128, D], F32, pds)
                ln2b = sb('ln2b', [128, D], F32, pds)
                dma('sp', ln2g[:], ln2g_d.to_broadcast([128, D]), (), ['ln2g'])
                dma('sp', ln2b[:], ln2b_d.to_broadcast([128, D]), (), ['ln2b'])
                ND = 6
                mhalfD = sb('mhalfD', [128, 4], F32, pds)
                memset('pool', mhalfD[:], -0.5, ['mhalfD'])
                yg = [[sb('yg%d_%d' % (i, jj), [128, D], F32, pds) for jj in range(4)] for i in range(ND)]
                bs = [sb('bsD%d' % i, [128, D], F32, pds) for i in range(ND)]
                stD = sb('stD', [128, ND, 12], F32, pds)
                smD = sb('smD', [128, ND, 8], F32, pds)
                all_ys = [('ys', e, sbk) for e in range(NE) for sbk in range(CB)]

                def d_load(t):
                    pt_ = t % ND
                    dma('sp', bs[pt_][:], base_d[t * 128:(t + 1) * 128, :], [('base', t)], [('bsD', pt_)])
                    for jj in range(4):
                        sc.dma('pool', (lambda e, t=t, jj=jj, pt_=pt_: e.indirect_dma_start(
                            out=yg[pt_][jj][:], out_offset=None, in_=ys_d[:, :],
                            in_offset=bass.IndirectOffsetOnAxis(slot4[:, t, jj:jj + 1], 0))),
                            r=(all_ys if t == 0 else []) + [('slot4', t)], w=[('yg', pt_, jj)])

                def d_part1(t):
                    pt_ = t % ND
                    y = yg[pt_]
                    kb = ('bsD', pt_)
                    for jj in range(4):
                        stt(bs[pt_][:], y[jj][:], gate4[:, t, jj:jj + 1], bs[pt_][:], ALU.mult, ALU.add,
                            [('yg', pt_, jj), ('gate4', t)], [kb])
                    src = bs[pt_]
                    sc.op('dve', (lambda e, src=src, pt_=pt_: e.bn_stats(stD[:, pt_, 0:6], src[:, 0:512])), r=[kb], w=[('stD0', pt_)])
                    sc.op('dve', (lambda e, src=src, pt_=pt_: e.bn_stats(stD[:, pt_, 6:12], src[:, 512:1024])), r=[kb], w=[('stD1', pt_)])
                    sc.op('dve', (lambda e, pt_=pt_: e.bn_aggr(smD[:, pt_, 0:2], stD[:, pt_, 0:12])),
                          r=[('stD0', pt_), ('stD1', pt_)], w=[('smD', pt_)])
                    ts('dve', smD[:, pt_, 2:3], smD[:, pt_, 1:2], 1.0, EPS, ALU.mult, ALU.add, [('smD', pt_)], [('smD2', pt_)])
                    tt('pool', smD[:, pt_, 3:4], smD[:, pt_, 2:3], mhalfD[:, 0:1], ALU.pow,
                       [('smD2', pt_), 'mhalfD'], [('smD3', pt_)])
                    ts('pool', smD[:, pt_, 4:5], smD[:, pt_, 0:1], smD[:, pt_, 3:4], -1.0, ALU.mult, ALU.mult,
                       [('smD', pt_), ('smD3', pt_)], [('smD4', pt_)])
                    act(y[0][:], bs[pt_][:], AF.Identity, [kb, ('smD3', pt_), ('smD4', pt_)], [('yg', pt_, 0)],
                        bias=smD[:, pt_, 4:5], scale=smD[:, pt_, 3:4])

                def d_part2(t):
                    pt_ = t % ND
                    y = yg[pt_]
                    tt('dve', y[0][:], y[0][:], ln2g[:], ALU.mult, ['ln2g'], [('yg', pt_, 0)])
                    tt('dve', y[0][:], y[0][:], ln2b[:], ALU.add, ['ln2b'], [('yg', pt_, 0)])
                    dma('sp', out_d[t * 128:(t + 1) * 128, :], y[0][:], [('yg', pt_, 0)], [('out', t)])

                PF = ND - 2
                for t in range(min(PF, NT)):
                    d_load(t)
                for t in range(NT):
                    if t + PF < NT:
                        d_load(t + PF)
                    d_part1(t)
                    if t >= 1:
                        d_part2(t - 1)
                d_part2(NT - 1)
            sc.wait_keys('sp', [('out', t) for t in range(NT)])

        if dbg == 'B':
            sc.wait_keys('sp', [('dbgout', t) for t in range(NT)])

        sc.wait_keys('sp', ['outfinal'])

        with nc.Block() as block:
            @block.tensor
            def _(e):
                sc.replay('pe', e)

            @block.scalar
            def _(e):
                sc.replay('act', e)

            @block.vector
            def _(e):
                sc.replay('dve', e)

            @block.gpsimd
            def _(e):
                sc.replay('pool', e)

            @block.sync
            def _(e):
                sc.replay('sp', e)
    return nc


def make_consts(S, C):
    NKT = S // 128
    ident = np.eye(128, dtype=np.float32)
    tri = (np.arange(128)[:, None] < np.arange(128)[None, :]).astype(np.float32)
    kp = np.arange(128)[:, None]
    qp = np.arange(128)[None, :]
    m1 = np.where(kp > qp, NEG, 0.0).astype(np.float32)
    mask = np.tile(m1, (1, 4))
    slopes = np.array([2.0 ** (-8.0 * (h + 1) / 4) for h in range(4)], dtype=np.float64)
    alibi = np.zeros((128, 4, NKT), dtype=np.float64)
    for h in range(4):
        for dd in range(NKT):
            alibi[:, h, dd] = slopes[h] * (np.arange(128) - 128.0 * dd)
    alibi = alibi.reshape(128, 4 * NKT).astype(np.float32)
    ratio = np.zeros((128, 4, 16), dtype=np.float32)
    for g, w in enumerate((2, 4, 8, 16)):
        for t in range(16):
            ratio[:, g, t] = float(w) / float(min(t + 1, w))
    ratio = ratio.reshape(128, 64)
    carry0 = np.tile((np.arange(32, dtype=np.float32) * C)[None, :], (128, 1))
    band = np.zeros((128, 4, 3, 128), dtype=np.float64)
    tp_ = np.arange(128)[:, None]
    t_ = np.arange(128)[None, :]
    for g, w in enumerate((2, 4, 8, 16)):
        dlt = t_ - tp_
        band[:, g, 0, :] = np.where((dlt >= 0) & (dlt < w), 1.0 / w, 0.0) - (dlt == 0)
        band[:, g, 1, :] = np.where((dlt + 128 > 0) & (dlt + 128 < w), 1.0 / w, 0.0)
        cntt = np.minimum(t_ + 1, w).astype(np.float64)
        band[:, g, 2, :] = np.where((dlt >= 0) & (dlt < w), 1.0 / cntt, 0.0) - (dlt == 0)
    band = band.reshape(128, 4 * 3 * 128).astype(np.float32)
    return dict(c_ident=ident, c_tri=tri, c_mask=mask, c_alibi=alibi, c_ratio=ratio, c_carry0=carry0, c_band=band)


def make_in_maps(inputs, cfg, ncores):
    S, NSEQ, C = cfg['S'], cfg['NSEQ'], cfg['C']
    f = lambda a: np.ascontiguousarray(np.asarray(a, dtype=np.float32))
    x = f(inputs['x'])
    p = f(inputs['p'])[0]
    shared = dict(
        w_in=f(inputs['w_in'])[0],
        b_gate=f(f(inputs['b_gate'])[0].reshape(16, 128).T),
        lam=f(np.concatenate([np.asarray(inputs[k], dtype=np.float32)[0] for k in
                              ('lambda_q1', 'lambda_k1', 'lambda_q2', 'lambda_k2')]).reshape(1, 256)),
        subln_g=f(inputs['subln_g']).reshape(1, 128),
        w_attn_br=f(inputs['w_attn_br'])[0],
        w_pool_mix=f(inputs['w_pool_mix'])[0],
        pool_scale=f(f(inputs['pool_scale'])[0].reshape(4, 128).T),
        w_pool_br=f(inputs['w_pool_br'])[0],
        w_out=f(inputs['w_out'])[0],
        ln1_g=f(inputs['ln1_g']).reshape(1, D),
        ln1_b=f(inputs['ln1_b']).reshape(1, D),
        w_router=f(inputs['w_router'])[0],
        b_router=f(inputs['b_router']).reshape(1, 32),
        w_up=f(inputs['w_up'])[0],
        b_up=f(f(inputs['b_up'])[0].reshape(32, 16, 128).transpose(2, 0, 1).reshape(128, 512)),
        w_down=f(inputs['w_down'])[0],
        b_down=f(inputs['b_down'])[0],
        w_ple_gate=f(inputs['w_ple_gate'])[0],
        w_ple_proj=f(inputs['w_ple_proj'])[0],
        ln2_g=f(inputs['ln2_g']).reshape(1, D),
        ln2_b=f(inputs['ln2_b']).reshape(1, D),
    )
    shared.update(make_consts(S, C))
    maps = []
    for c in range(ncores):
        m = dict(shared)
        m['x'] = f(x[c * NSEQ:(c + 1) * NSEQ].reshape(NSEQ * S, D))
        m['p'] = f(p[c * NSEQ:(c + 1) * NSEQ].reshape(NSEQ * S, 256))
        maps.append(m)
    return maps


FULL_CFG = dict(S=4096, NSEQ=2, C=1280)


def kernel(**inputs):
    cfg = dict(FULL_CFG)
    nc = build(cfg)
    maps = make_in_maps(inputs, cfg, NCORES)
    res = run_bass_kernel_spmd(nc, maps, core_ids=list(range(NCORES)))
    outs = [np.asarray(r["out"], dtype=np.float32).reshape(cfg['NSEQ'], cfg['S'], D) for r in res.results]
    return np.concatenate(outs, axis=0)
```

```python
import math
from contextlib import ExitStack
import numpy as np
import concourse.bass as bass
import concourse.mybir as mybir
from concourse.bass_utils import run_bass_kernel_spmd

F32 = mybir.dt.float32
BF16 = mybir.dt.bfloat16
I32 = mybir.dt.int32
AF = mybir.ActivationFunctionType
ALU = mybir.AluOpType

D = 1024
NCORES = 8
ALPHA = 2.0 ** 0.25
LAM_INIT = 0.2
EPS = 1e-5
NEG = -30000.0


class Queue:
    def __init__(self, name, sem, ring):
        self.name = name
        self.sem = sem
        self.count = 0
        self.seen = {}
        self.ops = []
        self.ring = ring
        self.ring_val = [0] * len(ring)
        self.dma_n = 0


class Sched:
    def __init__(self, nc):
        self.nc = nc
        self.q = {}
        self.writers = {}
        self.readers = {}
        self.sems = {}

    def add_queue(self, name, sem, ring=()):
        self.q[name] = Queue(name, sem, list(ring))
        if sem is not None:
            self.sems[id(sem)] = sem
        for s in ring:
            self.sems[id(s)] = s

    def _deps(self, q, r, w):
        deps = {}

        def add(tok):
            sid, val = tok
            if q.name == 'pe' and q.sem is not None and sid == id(q.sem):
                return
            if deps.get(sid, 0) < val:
                deps[sid] = val
        for k in r:
            for tok in self.writers.get(k, {}).items():
                add(tok)
        for k in w:
            for tok in self.writers.get(k, {}).items():
                add(tok)
            for tok in self.readers.get(k, {}).items():
                add(tok)
        waits = []
        for sid, val in deps.items():
            if q.seen.get(sid, 0) < val:
                q.seen[sid] = val
                waits.append((self.sems[sid], val))
        return waits

    def _commit(self, tok, r, w):
        sid, val = tok
        for k in r:
            d = self.readers.setdefault(k, {})
            if d.get(sid, 0) < val:
                d[sid] = val
        for k in w:
            self.writers[k] = {sid: val}
            self.readers[k] = {}

    def op(self, qn, fn, r=(), w=(), signal=True, extra_waits=()):
        q = self.q[qn]
        waits = self._deps(q, r, w)
        for tok in extra_waits:
            sid, val = tok
            if q.seen.get(sid, 0) < val:
                q.seen[sid] = val
                waits.append((self.sems[sid], val))
        if signal:
            q.count += 1
            tok = (id(q.sem), q.count)
            q.ops.append((waits, fn, (q.sem, 1)))
        else:
            tok = (id(q.sem), q.count + 1)
            q.ops.append((waits, fn, None))
        self._commit(tok, r, w)
        return tok

    def dma(self, qn, fn, r=(), w=()):
        q = self.q[qn]
        slot = q.dma_n % len(q.ring)
        q.dma_n += 1
        sem = q.ring[slot]
        waits = self._deps(q, r, w)
        prev = q.ring_val[slot]
        if prev > 0 and q.seen.get(id(sem), 0) < prev:
            q.seen[id(sem)] = prev
            waits.append((sem, prev))
        q.ring_val[slot] = prev + 16
        tok = (id(sem), prev + 16)
        q.ops.append((waits, fn, (sem, 16)))
        self._commit(tok, r, w)
        return tok

    def wait_keys(self, qn, keys):
        q = self.q[qn]
        waits = self._deps(q, (), keys)
        q.ops.append((waits, None, None))

    def barrier(self):
        toks = []
        for q in self.q.values():
            if q.sem is not None and q.count > 0:
                toks.append((id(q.sem), q.count))
            for s, v in zip(q.ring, q.ring_val):
                if v > 0:
                    toks.append((id(s), v))
        for q in self.q.values():
            waits = []
            for sid, val in toks:
                if q.name == 'pe' and q.sem is not None and sid == id(q.sem):
                    continue
                if q.seen.get(sid, 0) < val:
                    q.seen[sid] = val
                    waits.append((self.sems[sid], val))
            q.ops.append((waits, None, None))

    def replay(self, qn, eng):
        for waits, fn, inc in self.q[qn].ops:
            for sem, val in waits:
                eng.wait_ge(sem, val)
            if fn is not None:
                ins = fn(eng)
                if inc is not None:
                    ins.then_inc(inc[0], inc[1])


def build(cfg):
    S = cfg['S']
    NSEQ = cfg['NSEQ']
    C = cfg['C']
    NE = cfg.get('NE', 32)
    phases = cfg.get('phases', 'ABCD')
    dbg = cfg.get('dbg', False)
    T = S * NSEQ
    NCH = S // 512
    NKT = S // 128
    NT = T // 128
    CB = C // 128
    NSLOT = NE * C

    nc = bass.Bass("TRN2", target_bir_lowering=False)

    def din(name, shape, dt=F32):
        return nc.dram_tensor(name, list(shape), dt, kind="ExternalInput").ap()

    x_d = din("x", [T, D])
    p_d = din("p", [T, 256])
    w_in_d = din("w_in", [D, 4096])
    b_gate_d = din("b_gate", [128, 16])
    lam_d = din("lam", [1, 256])
    subln_d = din("subln_g", [1, 128])
    w_ab_d = din("w_attn_br", [512, D])
    w_mix_d = din("w_pool_mix", [4, 128, 128])
    pscale_d = din("pool_scale", [128, 4])
    w_pb_d = din("w_pool_br", [512, D])
    w_out_d = din("w_out", [D, D])
    ln1g_d = din("ln1_g", [1, D])
    ln1b_d = din("ln1_b", [1, D])
    w_r_d = din("w_router", [D, 32])
    b_r_d = din("b_router", [1, 32])
    w_up_d = din("w_up", [32, D, 2048])
    b_up_d = din("b_up", [128, 32 * 16])
    w_dn_d = din("w_down", [32, D, D])
    b_dn_d = din("b_down", [32, D])
    w_pg_d = din("w_ple_gate", [D, D])
    w_pp_d = din("w_ple_proj", [256, D])
    ln2g_d = din("ln2_g", [1, D])
    ln2b_d = din("ln2_b", [1, D])
    ident_d = din("c_ident", [128, 128])
    tri_d = din("c_tri", [128, 128])
    mask_d = din("c_mask", [128, 512])
    alibi_d = din("c_alibi", [128, 4 * NKT])
    ratio_d = din("c_ratio", [128, 64])
    carry0_d = din("c_carry0", [128, 32])
    band_d = din("c_band", [128, 4 * 3 * 128])

    out_d = nc.dram_tensor("out", [T, D], F32, kind="ExternalOutput").ap()
    atd = nc.dram_tensor("atd", [4, 128, T], BF16, kind="Internal").ap()
    xs_d = nc.dram_tensor("xs", [NSLOT + 128, D], BF16, kind="Internal").ap()
    ys_d = nc.dram_tensor("ys", [NSLOT + 128, D], F32, kind="Internal").ap()
    base_d = nc.dram_tensor("base", [T, D], F32, kind="Internal").ap()
    dbg_d = None
    if dbg:
        dbg_d = nc.dram_tensor("dbg", [T, D], F32, kind="ExternalOutput").ap()

    es = ExitStack()
    with es:
        def sb(name, shape, dt, stack=es):
            return stack.enter_context(nc.sbuf_tensor(name, list(shape), dt))

        def ps(name, shape, dt, stack=es):
            return stack.enter_context(nc.psum_tensor(name, list(shape), dt))

        def sem(name):
            return es.enter_context(nc.semaphore(name))

        sc = Sched(nc)
        sc.add_queue('pe', sem('s_pe'))
        sc.add_queue('act', sem('s_act'))
        sc.add_queue('dve', sem('s_dve'))
        sc.add_queue('pool', sem('s_pool'), [sem('r_pool%d' % i) for i in range(8)])
        sc.add_queue('sp', None, [sem('r_sp%d' % i) for i in range(8)])

        def mm(out, lhsT, rhs, start, stop, r, w, signal):
            return sc.op('pe', lambda e: e.matmul(out, lhsT=lhsT, rhs=rhs, start=start, stop=stop,
                                                  skip_group_check=True), r=r, w=w, signal=signal)

        def tr(out, in_, ident, r, w, signal):
            return sc.op('pe', lambda e: e.transpose(out, in_, ident), r=r, w=w, signal=signal)

        def act(out, in_, func, r, w, bias=None, scale=None, accum_out=None):
            kw = {}
            if bias is not None:
                kw['bias'] = bias
            if scale is not None:
                kw['scale'] = scale
            if accum_out is not None:
                kw['accum_out'] = accum_out
            return sc.op('act', lambda e: e.activation(out, in_, func, **kw), r=r, w=w)

        def ts(qn, out, in0, s1, s2, op0, op1, r, w, accum_out=None):
            kw = {}
            if accum_out is not None:
                kw['accum_out'] = accum_out
            if op1 is None:
                return sc.op(qn, lambda e: e.tensor_scalar(out, in0, s1, None, op0, **kw), r=r, w=w)
            return sc.op(qn, lambda e: e.tensor_scalar(out, in0, s1, s2, op0, op1, **kw), r=r, w=w)

        def tt(qn, out, in0, in1, op, r, w):
            return sc.op(qn, lambda e: e.tensor_tensor(out, in0, in1, op), r=r, w=w)

        def stt(out, in0, scalar, in1, op0, op1, r, w, accum_out=None):
            kw = {}
            if accum_out is not None:
                kw['accum_out'] = accum_out
            return sc.op('dve', lambda e: e.scalar_tensor_tensor(out, in0, scalar, in1, op0, op1, **kw),
                         r=r, w=w)

        def cp(qn, out, in_, r, w):
            return sc.op(qn, lambda e: e.tensor_copy(out, in_), r=r, w=w)

        def memset(qn, ap, val, w):
            return sc.op(qn, lambda e: e.memset(ap, val), r=(), w=w)

        def dma(qn, out, in_, r, w):
            return sc.dma(qn, lambda e: e.dma_start(out=out, in_=in_), r=r, w=w)

        banks = [ps('bank%d' % i, [128, 512], F32) for i in range(8)]

        def bk(i):
            return ('ps', i)

        identF = sb('identF', [128, 128], F32)
        identB = sb('identB', [128, 128], BF16)
        dma('sp', identF[:], ident_d[:, :], (), ['identF'])
        dma('pool', identB[:], ident_d[:, :], (), ['identB'])

        if 'A' in phases:
            pa = ExitStack()
            with pa:
                Wqk = sb('Wqk', [128, 8, 1024], BF16, pa)
                Wv = sb('Wv', [128, 8, 512], BF16, pa)
                w_in_v = w_in_d.rearrange("(kc p) n -> p kc n", p=128)
                for kc in range(8):
                    dma('pool', Wqk[:, kc, :], w_in_v[:, kc, 0:1024], (), [('Wqk', kc)])
                    dma('pool', Wv[:, kc, :], w_in_v[:, kc, 1024:1536], (), [('Wv', kc)])
                wqk_keys = [('Wqk', kc) for kc in range(8)]
                wv_keys = [('Wv', kc) for kc in range(8)]
                maskB = sb('maskB', [128, 512], BF16, pa)
                dma('pool', maskB[:], mask_d[:, :], (), ['maskB'])
                alibi = sb('alibi', [128, 4 * NKT], F32, pa)
                dma('sp', alibi[:], alibi_d[:, :], (), ['alibi'])
                g2 = sb('g2', [128, 128], F32, pa)
                dma('sp', g2[:], subln_d.to_broadcast([128, 128]), (), ['g2'])
                lamt = sb('lamt', [128, 256], F32, pa)
                dma('sp', lamt[:], lam_d.to_broadcast([128, 256]), (), ['lamt'])
                lamw = sb('lamw', [128, 8], F32, pa)
                junk64 = sb('junk64', [128, 64], F32, pa)
                stt(junk64[:], lamt[:, 0:64], 1.0, lamt[:, 64:128], ALU.mult, ALU.mult, ['lamt'], ['junk64', 'lamw'],
                    accum_out=lamw[:, 0:1])
                stt(junk64[:], lamt[:, 128:192], 1.0, lamt[:, 192:256], ALU.mult, ALU.mult, ['lamt'], ['junk64', 'lamw'],
                    accum_out=lamw[:, 1:2])
                act(lamw[:, 2:4], lamw[:, 0:2], AF.Exp, ['lamw'], ['lamw'])
                stt(lamw[:, 4:5], lamw[:, 3:4], -LAM_INIT, lamw[:, 2:3], ALU.add, ALU.subtract, ['lamw'], ['lamw'])
                ts('dve', g2[:], g2[:], 1.0 - LAM_INIT, None, ALU.mult, None, ['g2'], ['g2'])

                KT = sb('KT', [128, 4, S], BF16, pa)
                Va = sb('Va', [128, NKT, 4, 129], BF16, pa)
                memset('pool', Va[:, :, :, 128:129], 1.0, ['Va1'])
                xin = [sb('xinA%d' % i, [128, 4, D], BF16, pa) for i in range(2)]
                XT = [sb('XTA%d' % i, [128, 8, 512], BF16, pa) for i in range(2)]
                mhalf = sb('mhalfA', [128, 4], F32, pa)
                memset('pool', mhalf[:], -0.5, ['mhalfA'])
                QT = [sb('QT%d' % i, [128, 4, 512], BF16, pa) for i in range(2)]
                NEB = 8
                Eb = [sb('Eb%d' % i, [128, 512], BF16, pa) for i in range(NEB)]
                Om = [sb('Om%d' % i, [128, 4, 128], F32, pa) for i in range(2)]
                rs = sb('rs', [128, 16], F32, pa)
                raw = sb('rawA', [128, 2, 2, 258], F32, pa)
                odiff = sb('odiff', [128, 4, 128], F32, pa)
                osq = sb('osq', [128, 128], F32, pa)
                onb = sb('onb', [128, 4, 128], BF16, pa)
                ATc = [sb('ATc%d' % i, [128, 4, 512], BF16, pa) for i in range(2)]
                ebn = [0]
                pend_epi = [None]
                pbn = [0]

                def pbank():
                    b = pbn[0] % 8
                    pbn[0] += 1
                    return b

                def sbank(m, dd):
                    return (2 + m) if dd % 2 == 0 else m

                def run_epi():
                    f = pend_epi[0]
                    pend_epi[0] = None
                    if f is not None:
                        f()

                for s in range(NSEQ):
                    for j in range(NCH):
                        ci = s * NCH + j
                        b2 = ci % 2
                        t0 = s * S + j * 512
                        dma('pool', xin[b2][:], x_d[t0:t0 + 512, :].rearrange("(tt p) d -> p tt d", p=128),
                            (), [('xinA', b2)])
                        for kc in range(8):
                            bnk = pbank()
                            tbk = banks[bnk][:].bitcast(BF16)
                            for tt_ in range(4):
                                tr(tbk[:, tt_ * 128:(tt_ + 1) * 128], xin[b2][:, tt_, kc * 128:(kc + 1) * 128],
                                   identB[:], [('xinA', b2), 'identB'], [bk(bnk)], signal=(tt_ == 3))
                            cp('dve', XT[b2][:, kc, :], tbk[:, 0:512], [], [bk(bnk), ('XTA', b2, kc)])
                        xt_keys = [('XTA', b2, kc) for kc in range(8)]
                        for c in range(8):
                            bnk = pbank()
                            for kc in range(8):
                                mm(banks[bnk][:], Wqk[:, kc, c * 128:(c + 1) * 128], XT[b2][:, kc, :],
                                   kc == 0, kc == 7, [('Wqk', kc), ('XTA', b2, kc)], [bk(bnk)], signal=(kc == 7))
                            if c < 4:
                                dst, dk = QT[b2][:, c, :], ('QT', b2, c)
                            else:
                                dst, dk = KT[:, c - 4, j * 512:(j + 1) * 512], ('KT', c - 4, j)
                            cp('dve', dst, banks[bnk][:], [], [bk(bnk), dk])
                        for tt_ in range(4):
                            bnk = pbank()
                            for kc in range(8):
                                mm(banks[bnk][:], XT[b2][:, kc, tt_ * 128:(tt_ + 1) * 128], Wv[:, kc, :],
                                   kc == 0, kc == 7, [('Wv', kc), ('XTA', b2, kc)], [bk(bnk)], signal=(kc == 7))
                            kt = j * 4 + tt_
                            dst = Va[:, kt, :, 0:128]
                            src = banks[bnk][:].rearrange("p (h d) -> p h d", h=4)
                            cp('dve', dst, src, [], [bk(bnk), ('Va', kt)])
                        for h in range(4):
                            accb = [[banks[4 + 2 * m], banks[5 + 2 * m]] for m in range(2)]
                            acck = [[bk(4 + 2 * m), bk(5 + 2 * m)] for m in range(2)]
                            ndd = min(4 * j + 4, (6, 18, 1 << 30, 1 << 30)[h])
                            pendq = []

                            def pv(dd, i0, ebs):
                                for m in range(2):
                                    for i in range(i0, 4):
                                        kt = 4 * j + i - dd
                                        a = accb[m][i // 2][:, (i % 2) * 129:(i % 2) * 129 + 129]
                                        mm(a, Eb[ebs[m]][:, i * 128:(i + 1) * 128], Va[:, kt, h, :],
                                           (dd == 0 and i % 2 == 0), False,
                                           [('Eb', ebs[m]), ('Va', kt), 'Va1'], [acck[m][i // 2]],
                                           signal=(i == 3))

                            for dd in range(ndd):
                                i0 = max(0, dd - 4 * j)
                                for i in range(i0, 4):
                                    kt = 4 * j + i - dd
                                    last = (i == 3) and dd != 0
                                    for m in range(2):
                                        pb = m * 64
                                        sbk = sbank(m, dd)
                                        mm(banks[sbk][:, i * 128:(i + 1) * 128],
                                           KT[pb:pb + 64, h, kt * 128:(kt + 1) * 128],
                                           QT[b2][pb:pb + 64, h, i * 128:(i + 1) * 128],
                                           i == i0, False, [('KT', h, kt // 4), ('QT', b2, h)], [bk(sbk)],
                                           signal=last)
                                if dd == 0:
                                    for m in range(2):
                                        sbk = sbank(m, dd)
                                        mm(banks[sbk][:], identB[:], maskB[:], False, True,
                                           ['identB', 'maskB'], [bk(sbk)], signal=True)
                                bcol = h * NKT + dd
                                ebs = []
                                for m in range(2):
                                    eb = ebn[0] % NEB
                                    ebn[0] += 1
                                    ebs.append(eb)
                                    sbk = sbank(m, dd)
                                    act(Eb[eb][:, i0 * 128:512], banks[sbk][:, i0 * 128:512], AF.Exp,
                                        ['alibi'], [bk(sbk), ('Eb', eb)],
                                        bias=alibi[:, bcol:bcol + 1], scale=0.125)
                                pendq.append((dd, i0, ebs))
                                if len(pendq) > 2:
                                    pv(*pendq.pop(0))
                                if dd == min(9, ndd - 1):
                                    run_epi()
                            while pendq:
                                pv(*pendq.pop(0))
                            for m in range(2):
                                for hb_ in range(2):
                                    rk = ('raw', m, hb_)
                                    cp('dve', raw[:, m, hb_, :], accb[m][hb_][:, 0:258], [], [acck[m][hb_], rk])
                            for m in range(2):
                                for i in range(4):
                                    o = (i % 2) * 129
                                    col = m * 4 + i
                                    rk = ('raw', m, i // 2)
                                    sc.op('dve', (lambda e, m=m, i=i, o=o, col=col: e.reciprocal(
                                        rs[:, col:col + 1], raw[:, m, i // 2, o + 128:o + 129])),
                                          r=[rk], w=[('rs', col)])
                                    ts('dve', Om[m][:, i, :], raw[:, m, i // 2, o:o + 128], rs[:, col:col + 1], None,
                                       ALU.mult, None, [rk, ('rs', col)], [('Om', m, i)])
                            def make_epi(h=h, b2=b2, ci=ci, t0=t0):
                                def epi():
                                    for i in range(4):
                                        stt(odiff[:, i, :], Om[1][:, i, :], lamw[:, 4:5], Om[0][:, i, :], ALU.mult, ALU.add,
                                            ['lamw', ('Om', 0, i), ('Om', 1, i)], [('odiff', i)])
                                        stt(osq[:], odiff[:, i, :], 1.0, odiff[:, i, :], ALU.mult, ALU.mult,
                                            [('odiff', i)], ['osq', ('rs', 8 + i)], accum_out=rs[:, 8 + i:9 + i])
                                    ts('pool', rs[:, 8:12], rs[:, 8:12], 1.0 / 128.0, EPS, ALU.mult, ALU.add,
                                       [('rs', 8 + i) for i in range(4)], [('rs', 8 + i) for i in range(4)])
                                    tt('pool', rs[:, 12:16], rs[:, 8:12], mhalf[:], ALU.pow,
                                       [('rs', 8 + i) for i in range(4)] + ['mhalfA'], [('rs', 12 + i) for i in range(4)])
                                    for i in range(4):
                                        stt(onb[:, i, :], odiff[:, i, :], rs[:, 12 + i:13 + i], g2[:], ALU.mult, ALU.mult,
                                            [('odiff', i), ('rs', 12 + i), 'g2'], [('onb', i)])
                                    tb = h % 2
                                    tbank = banks[tb][:].bitcast(BF16)
                                    for i in range(4):
                                        tr(tbank[:, i * 128:(i + 1) * 128], onb[:, i, :], identB[:],
                                           [('onb', i), 'identB'], [bk(tb)], signal=(i == 3))
                                    cp('dve', ATc[b2][:, h, :], tbank[:, 0:512], [], [bk(tb), ('ATc', b2, h)])
                                    if h == 3:
                                        dma('sp', atd[:, :, t0:t0 + 512].rearrange("h p t -> p h t"), ATc[b2][:],
                                            [('ATc', b2, hh) for hh in range(4)], [('atd', ci)])
                                return epi
                            pend_epi[0] = make_epi()
                run_epi()
            sc.barrier()

        if dbg == 'A':
            with ExitStack() as pd:
                tmpb = sb('dbgb', [128, 4, T], BF16, pd)
                tmpf = sb('dbgf', [128, 4, T], F32, pd)
                dma('sp', tmpb[:], atd.rearrange("h p t -> p h t"), [('atd', ci) for ci in range(NSEQ * NCH)], ['dbgb'])
                cp('dve', tmpf[:], tmpb[:], ['dbgb'], ['dbgf'])
                dma('sp', dbg_d[0:512, 0:T].rearrange("(h p) t -> p h t", p=128), tmpf[:], ['dbgf'], ['dbgout'])
                sc.wait_keys('sp', ['dbgout'])
                sc.barrier()


        slot4 = sb('slot4', [128, NT, 4], I32)
        gate4 = sb('gate4', [128, NT, 4], F32)

        if 'B' in phases:
            pbs = ExitStack()
            with pbs:
                w_in_v = w_in_d.rearrange("(kc p) n -> p kc n", p=128)
                Wu = sb('Wu', [128, 8, 512], BF16, pbs)
                Wg = sb('Wg', [128, 8, 2048], BF16, pbs)
                Wab = sb('Wab', [128, 4, D], BF16, pbs)
                Wpb = sb('Wpb', [128, 4, D], BF16, pbs)
                Wmix = sb('Wmix', [128, 4, 128], BF16, pbs)
                Wout = sb('Wout', [128, 8, D], BF16, pbs)
                Wpg = sb('Wpg', [128, 8, D], BF16, pbs)
                Wpp = sb('Wpp', [128, 2, D], BF16, pbs)
                Wr = sb('Wr', [128, 8, 32], F32, pbs)
                for kc in range(8):
                    dma('pool', Wu[:, kc, :], w_in_v[:, kc, 1536:2048], (), [('Wu', kc)])
                for h in range(4):
                    dma('pool', Wmix[:, h, :], w_mix_d[h], (), [('Wmix', h)])
                for kc in range(8):
                    dma('pool', Wg[:, kc, :], w_in_v[:, kc, 2048:4096], (), [('Wg', kc)])
                for h in range(4):
                    dma('pool', Wab[:, h, :], w_ab_d[h * 128:(h + 1) * 128, :], (), [('Wab', h)])
                    dma('pool', Wpb[:, h, :], w_pb_d[h * 128:(h + 1) * 128, :], (), [('Wpb', h)])
                for kc in range(8):
                    dma('pool', Wout[:, kc, :], w_out_d[kc * 128:(kc + 1) * 128, :], (), [('Wout', kc)])
                for kc in range(8):
                    dma('pool', Wpg[:, kc, :], w_pg_d[kc * 128:(kc + 1) * 128, :], (), [('Wpg', kc)])
                for c2 in range(2):
                    dma('pool', Wpp[:, c2, :], w_pp_d[c2 * 128:(c2 + 1) * 128, :], (), [('Wpp', c2)])
                dma('sp', Wr[:], w_r_d.rearrange("(kc p) n -> p kc n", p=128), (), ['Wr'])
                bgate = sb('bgate', [128, 16], F32, pbs)
                dma('sp', bgate[:], b_gate_d[:, :], (), ['bgate'])
                pscale = sb('pscale', [128, 4], F32, pbs)
                dma('sp', pscale[:], pscale_d[:, :], (), ['pscale'])
                ln1g = sb('ln1g', [128, D], F32, pbs)
                ln1b = sb('ln1b', [128, D], F32, pbs)
                dma('sp', ln1g[:], ln1g_d.to_broadcast([128, D]), (), ['ln1g'])
                dma('sp', ln1b[:], ln1b_d.to_broadcast([128, D]), (), ['ln1b'])
                brt = sb('brt', [128, 32], F32, pbs)
                dma('sp', brt[:], b_r_d.to_broadcast([128, 32]), (), ['brt'])
                triB = sb('triB', [128, 128], BF16, pbs)
                dma('pool', triB[:], tri_d[:, :], (), ['triB'])
                onesB = sb('onesB', [128, 128], BF16, pbs)
                memset('pool', onesB[:], 1.0, ['onesB'])
                carry = sb('carry', [128, 32], F32, pbs)
                dma('sp', carry[:], carry0_d[:, :], (), ['carry'])

                Bc = sb('Bc', [128, 4, 3, 128], BF16, pbs)
                dma('pool', Bc[:].rearrange("p g k t -> p (g k t)"), band_d[:, :], (), ['Bc'])
                xin = [sb('xinB%d' % i, [128, D], F32, pbs) for i in range(2)]
                xbb = [sb('xbB%d' % i, [128, D], BF16, pbs) for i in range(2)]
                mhalfB = sb('mhalfB', [128, 4], F32, pbs)
                memset('pool', mhalfB[:], -0.5, ['mhalfB'])
                eB = sb('eB', [128, 4], F32, pbs)
                memset('pool', eB[:], math.e, ['eB'])
                XT = sb('XTB', [128, 8, 512], BF16, pbs)
                ATl = sb('ATl', [128, 4, 512], BF16, pbs)
                pin = sb('pin', [128, 4, 256], BF16, pbs)
                PinT = [sb('PinT%d' % i, [128, 2, 512], BF16, pbs) for i in range(2)]
                U = sb('U', [128, 5, 512], BF16, pbs)
                PT = sb('PT', [128, 4, 512], BF16, pbs)
                MT = sb('MT', [128, 4, 512], BF16, pbs)
                sag = [[sb('sag%d%d' % (i, k), [128, 512], F32, pbs) for k in range(2)] for i in range(2)]
                mixT = [sb('mixT%d' % i, [128, 8, 512], BF16, pbs) for i in range(2)]
                Y = [sb('Y%d' % i, [128, D], F32, pbs) for i in range(3)]
                X1 = [sb('X1_%d' % i, [128, D], F32, pbs) for i in range(2)]
                x1b = [sb('x1b%d' % i, [128, D], BF16, pbs) for i in range(2)]
                X1T32 = sb('X1T32', [128, 8, 128], F32, pbs)
                X1Tb = sb('X1Tb', [128, 8, 128], BF16, pbs)
                stB = sb('stB', [128, 2, 12], F32, pbs)
                smB = sb('smB', [128, 2, 16], F32, pbs)
                Lr = sb('Lr', [128, 32], F32, pbs)
                V8 = sb('V8', [128, 8], F32, pbs)
                Mb = sb('Mb', [128, 32], BF16, pbs)
                slotf = sb('slotf', [128, 32], F32, pbs)
                junk32 = sb('junk32', [128, 32], F32, pbs)
                s4f = sb('s4f', [128, 4], F32, pbs)
                e4 = sb('e4', [128, 4], F32, pbs)
                nbk = [0]

                held = set()

                def nb():
                    while True:
                        b = nbk[0] % 8
                        nbk[0] += 1
                        if b not in held:
                            return b

                NCI = NSEQ * NCH
                xn = [0]

                def y_load(gt_):
                    dma('sp', Y[gt_ % 3][:], x_d[gt_ * 128:(gt_ + 1) * 128, :], (),
                        [('Y', gt_ % 3, 0), ('Y', gt_ % 3, 1)])

                def front_units(ci):
                    s, j = divmod(ci, NCH)
                    t0 = s * S + j * 512
                    mp = ci % 2
                    units = []

                    def u_x(tt_):
                        def f():
                            if tt_ == 0:
                                dma('sp', ATl[:], atd[:, :, t0:t0 + 512].rearrange("h p t -> p h t"), [('atd', ci)], ['ATl'])
                                dma('pool', pin[:], p_d[t0:t0 + 512, :].rearrange("(tt p) d -> p tt d", p=128), (), ['pin'])
                            xi = xn[0] % 2
                            xn[0] += 1
                            r0 = t0 + tt_ * 128
                            dma('sp', xin[xi][:], x_d[r0:r0 + 128, :], (), [('xinB', xi)])
                            act(xbb[xi][:], xin[xi][:], AF.Copy, [('xinB', xi)], [('xbB', xi)])
                            b = nb()
                            tbk = banks[b][:].bitcast(BF16)
                            for kc in range(8):
                                tr(tbk[:, kc * 128:(kc + 1) * 128], xbb[xi][:, kc * 128:(kc + 1) * 128],
                                   identB[:], [('xbB', xi), 'identB'], [bk(b)], signal=(kc == 7))
                            srcv = tbk[:, 0:1024].rearrange("p (k t) -> p k t", k=8)
                            if tt_ % 2 == 0:
                                cp('dve', XT[:, :, tt_ * 128:(tt_ + 1) * 128], srcv, [], [bk(b), ('XTB', tt_)])
                            else:
                                act(XT[:, :, tt_ * 128:(tt_ + 1) * 128], srcv, AF.Copy, [], [bk(b), ('XTB', tt_)])
                        return f

                    def u_u(tt_):
                        def f():
                            b = nb()
                            for kc in range(8):
                                mm(banks[b][:], XT[:, kc, tt_ * 128:(tt_ + 1) * 128], Wu[:, kc, :], kc == 0, kc == 7,
                                   [('Wu', kc), ('XTB', tt_)], [bk(b)], signal=(kc == 7))
                            if tt_ % 2 == 0:
                                act(U[:, 1 + tt_, :], banks[b][:], AF.Copy, [], [bk(b), ('U', 1 + tt_)])
                            else:
                                cp('dve', U[:, 1 + tt_, :], banks[b][:], [], [bk(b), ('U', 1 + tt_)])
                        return f

                    def u_p():
                        for c2 in range(2):
                            b = nb()
                            tbk = banks[b][:].bitcast(BF16)
                            for tt_ in range(4):
                                tr(tbk[:, tt_ * 128:(tt_ + 1) * 128], pin[:, tt_, c2 * 128:(c2 + 1) * 128],
                                   identB[:], ['pin', 'identB'], [bk(b)], signal=(tt_ == 3))
                            cp('dve', PinT[mp][:, c2, :], tbk[:, 0:512], [], [bk(b), ('PinT', mp, c2)])

                    def u_pool():
                        for g in range(4):
                            b = nb()
                            for tt_ in range(4):
                                o_ = banks[b][:, tt_ * 128:(tt_ + 1) * 128]
                                if j == 0 and tt_ == 0:
                                    mm(o_, U[:, 1, g * 128:(g + 1) * 128], Bc[:, g, 2, :], True, True,
                                       [('U', 1), 'Bc'], [bk(b)], signal=False)
                                else:
                                    mm(o_, U[:, 1 + tt_, g * 128:(g + 1) * 128], Bc[:, g, 0, :], True, False,
                                       [('U', 1 + tt_), 'Bc'], [bk(b)], signal=False)
                                    mm(o_, U[:, tt_, g * 128:(g + 1) * 128], Bc[:, g, 1, :], False, True,
                                       [('U', tt_), 'Bc'], [bk(b)], signal=(tt_ == 3))
                            if g % 2 == 0:
                                act(PT[:, g, :], banks[b][:], AF.Copy, [], [bk(b), ('PT', g)])
                            else:
                                cp('dve', PT[:, g, :], banks[b][:], [], [bk(b), ('PT', g)])
                        act(U[:, 0, :], U[:, 4, :], AF.Copy, [('U', 4)], [('U', 0)])
                        for g in range(4):
                            b = nb()
                            mm(banks[b][:], Wmix[:, g, :], PT[:, g, :], True, True, [('Wmix', g), ('PT', g)], [bk(b)],
                               signal=True)
                            act(MT[:, g, :], banks[b][:], AF.Copy, ['pscale'], [bk(b), ('MT', g)],
                                scale=pscale[:, g:g + 1])

                    def u_gate(c):
                        def f():
                            bGa, bGp, bA, bP = nb(), nb(), nb(), nb()
                            xtk = [('XTB', q_) for q_ in range(4)]
                            for kc in range(8):
                                mm(banks[bGa][:], Wg[:, kc, c * 128:(c + 1) * 128], XT[:, kc, :], kc == 0, kc == 7,
                                   [('Wg', kc)] + xtk, [bk(bGa)], signal=(kc == 7))
                            for kc in range(8):
                                mm(banks[bGp][:], Wg[:, kc, 1024 + c * 128:1024 + (c + 1) * 128], XT[:, kc, :],
                                   kc == 0, kc == 7, [('Wg', kc)] + xtk, [bk(bGp)], signal=(kc == 7))
                            for h in range(4):
                                mm(banks[bA][:], Wab[:, h, c * 128:(c + 1) * 128], ATl[:, h, :], h == 0, h == 3,
                                   [('Wab', h), 'ATl'], [bk(bA)], signal=(h == 3))
                            for g in range(4):
                                mm(banks[bP][:], Wpb[:, g, c * 128:(c + 1) * 128], MT[:, g, :], g == 0, g == 3,
                                   [('Wpb', g), ('MT', g)], [bk(bP)], signal=(g == 3))
                            sa, sp_ = sag[c % 2]
                            ka, kp_ = ('sag', c % 2, 0), ('sag', c % 2, 1)
                            act(sa[:], banks[bGa][:], AF.Sigmoid, ['bgate'], [bk(bGa), ka], bias=bgate[:, c:c + 1])
                            act(sp_[:], banks[bGp][:], AF.Sigmoid, ['bgate'], [bk(bGp), kp_],
                                bias=bgate[:, 8 + c:9 + c])
                            tt('dve', sa[:], sa[:], banks[bA][:], ALU.mult, [], [ka, bk(bA)])
                            tt('dve', sp_[:], sp_[:], banks[bP][:], ALU.mult, [], [kp_, bk(bP)])
                            tt('dve', mixT[mp][:, c, :], sa[:], sp_[:], ALU.add, [ka, kp_], [('mixT', mp, c)])
                        return f

                    units += [u_x(tt_) for tt_ in range(4)]
                    units += [u_u(tt_) for tt_ in range(4)]
                    units += [u_p, u_pool]
                    units += [u_gate(c) for c in range(8)]
                    return units

                def tail_units(ci):
                    s, j = divmod(ci, NCH)
                    t0 = s * S + j * 512
                    mp = ci % 2

                    def W_a(tt_, ci=ci, t0=t0):
                        pb_ = tt_ % 2
                        yb_ = ((t0 // 128) + tt_) % 3
                        r0 = t0 + tt_ * 128
                        gt_ = (t0 // 128) + tt_
                        if gt_ == 0:
                            y_load(0)
                        if gt_ + 1 < NT:
                            y_load(gt_ + 1)
                        b0, b1 = nb(), nb()
                        for half, b in ((0, b0), (1, b1)):
                            for kc in range(8):
                                mm(banks[b][:], mixT[mp][:, kc, tt_ * 128:(tt_ + 1) * 128],
                                   Wout[:, kc, half * 512:(half + 1) * 512], kc == 0, kc == 7,
                                   [('mixT', mp, kc), ('Wout', kc)], [bk(b)], signal=(kc == 7))
                            hs = slice(half * 512, (half + 1) * 512)
                            stt(Y[yb_][:, hs], Y[yb_][:, hs], ALPHA, banks[b][:], ALU.mult, ALU.add, [],
                                [bk(b), ('Y', yb_, half)])

                    def W_b(tt_, ci=ci, t0=t0):
                        pb_ = tt_ % 2
                        yb_ = ((t0 // 128) + tt_) % 3
                        src, dst = Y[yb_], X1[pb_]
                        yk = [('Y', yb_, 0), ('Y', yb_, 1)]
                        sc.op('dve', (lambda e, src=src, pb_=pb_: e.bn_stats(stB[:, pb_, 0:6], src[:, 0:512])),
                              r=[yk[0]], w=[('stB0', pb_)])
                        sc.op('dve', (lambda e, src=src, pb_=pb_: e.bn_stats(stB[:, pb_, 6:12], src[:, 512:1024])),
                              r=[yk[1]], w=[('stB1', pb_)])
                        sc.op('dve', (lambda e, pb_=pb_: e.bn_aggr(smB[:, pb_, 0:2], stB[:, pb_, 0:12])),
                              r=[('stB0', pb_), ('stB1', pb_)], w=[('smB', pb_)])
                        ts('dve', smB[:, pb_, 2:3], smB[:, pb_, 1:2], 1.0, EPS, ALU.mult, ALU.add, [('smB', pb_)],
                           [('smB2', pb_)])
                        tt('pool', smB[:, pb_, 3:4], smB[:, pb_, 2:3], mhalfB[:, 0:1], ALU.pow,
                           [('smB2', pb_), 'mhalfB'], [('smB3', pb_)])
                        ts('dve', dst[:], src[:], smB[:, pb_, 0:1], smB[:, pb_, 3:4], ALU.subtract, ALU.mult,
                           yk + [('smB', pb_), ('smB3', pb_)], [('X1', pb_)])
                        tt('dve', dst[:], dst[:], ln1g[:], ALU.mult, ['ln1g'], [('X1', pb_)])
                        tt('dve', dst[:], dst[:], ln1b[:], ALU.add, ['ln1b'], [('X1', pb_)])
                        if dbg == 'B':
                            tile = (t0 // 128) + tt_
                            dma('sp', dbg_d[tile * 128:(tile + 1) * 128, :], dst[:], [('X1', pb_)], [('dbgout', tile)])

                    tl_state = {}

                    def Tl_a(tt_, t0=t0, ci=ci):
                        pb_ = tt_ % 2
                        tile = (t0 // 128) + tt_
                        X1c = X1[pb_]
                        kx = ('X1', pb_)
                        for hb in range(2):
                            b = nb()
                            for k4 in range(4):
                                kc = hb * 4 + k4
                                tr(banks[b][:, k4 * 128:(k4 + 1) * 128], X1c[:, kc * 128:(kc + 1) * 128], identF[:],
                                   [kx, 'identF'], [bk(b)], signal=(k4 == 3))
                            act(X1T32[:, hb * 4:(hb + 1) * 4, :],
                                banks[b][:].rearrange("p (k t) -> p k t", k=4), AF.Copy, [], [bk(b), ('X1T32', hb)])
                            cp('dve', X1Tb[:, hb * 4:(hb + 1) * 4, :],
                               banks[b][:].rearrange("p (k t) -> p k t", k=4), [], [bk(b), ('X1Tb', hb)])
                        bR = nb()
                        for kc in range(8):
                            mm(banks[bR][:, 0:32], X1T32[:, kc, :], Wr[:, kc, :], kc == 0, kc == 7,
                               [('X1T32', kc // 4), 'Wr'], [bk(bR)], signal=(kc == 7))
                        pbanks = []
                        for half in range(2):
                            bG, bPp = nb(), nb()
                            pbanks.append((bG, bPp))
                            for kc in range(8):
                                mm(banks[bG][:], X1Tb[:, kc, :], Wpg[:, kc, half * 512:(half + 1) * 512],
                                   kc == 0, kc == 7, [('X1Tb', kc // 4), ('Wpg', kc)], [bk(bG)], signal=(kc == 7))
                            for c2 in range(2):
                                mm(banks[bPp][:], PinT[ci % 2][:, c2, tt_ * 128:(tt_ + 1) * 128],
                                   Wpp[:, c2, half * 512:(half + 1) * 512], c2 == 0, c2 == 1,
                                   [('PinT', ci % 2, c2), ('Wpp', c2)], [bk(bPp)], signal=(c2 == 1))
                        yb_ = tile % 3
                        Yc = Y[yb_]
                        Ptmp = X1T32[:].rearrange("p k t -> p (k t)")
                        for half in range(2):
                            bG, bPp = pbanks[half]
                            hs = slice(half * 512, (half + 1) * 512)
                            act(Yc[:, hs], banks[bG][:], AF.Sigmoid, [], [bk(bG), ('Y', yb_, half)])
                            act(Ptmp[:, hs], banks[bPp][:], AF.Copy, [], [bk(bPp), ('X1T32', half)])
                        tt('dve', Lr[:], banks[bR][:, 0:32], brt[:], ALU.add, ['brt'], [bk(bR), 'Lr'])
                        sc.op('dve', lambda e: e.max(out=V8[:], in_=Lr[:]), r=['Lr'], w=['V8'])
                        ts('dve', Mb[:], Lr[:], V8[:, 3:4], None, ALU.is_ge, None, ['Lr', 'V8'], ['Mb'])
                        bK = nb()
                        mm(banks[bK][:, 0:32], triB[:], Mb[:], True, False, ['triB', 'Mb'], [bk(bK)], signal=False)
                        mm(banks[bK][:, 32:64], onesB[:], Mb[:], False, True, ['onesB', 'Mb'], [bk(bK)], signal=True)
                        tt('dve', slotf[:], banks[bK][:, 0:32], carry[:], ALU.add, ['carry'], [bk(bK), 'slotf'])
                        tt('dve', carry[:], carry[:], banks[bK][:, 32:64], ALU.add, [], [bk(bK), 'carry'])
                        for jj in range(4):
                            stt(junk32[:], Lr[:], V8[:, jj:jj + 1], slotf[:], ALU.is_equal, ALU.mult,
                                ['Lr', 'V8', 'slotf'], ['junk32', ('s4f', jj)], accum_out=s4f[:, jj:jj + 1])
                        cp('dve', slot4[:, tile, :], s4f[:], [('s4f', jj) for jj in range(4)], [('slot4', tile)])

                    def Tl_b(tt_, t0=t0):
                        pb_ = tt_ % 2
                        tile = (t0 // 128) + tt_
                        yb_ = tile % 3
                        X1c, Yc = X1[pb_], Y[yb_]
                        kx = ('X1', pb_)
                        ts('dve', e4[:], V8[:, 0:4], V8[:, 0:1], None, ALU.subtract, None, ['V8'], ['e4'])
                        tt('pool', e4[:], eB[:], e4[:], ALU.pow, ['eB'], ['e4'])
                        Ptmp = X1T32[:].rearrange("p k t -> p (k t)")
                        for half in range(2):
                            hs = slice(half * 512, (half + 1) * 512)
                            yk = ('Y', yb_, half)
                            tt('dve', Yc[:, hs], Yc[:, hs], Ptmp[:, hs], ALU.mult, [('X1T32', half)], [yk])
                            stt(Yc[:, hs], X1c[:, hs], ALPHA, Yc[:, hs], ALU.mult, ALU.add, [kx], [yk])
                        dma('sp', base_d[tile * 128:(tile + 1) * 128, :], Yc[:],
                            [('Y', yb_, 0), ('Y', yb_, 1)], [('base', tile)])
                        ts('dve', junk32[:, 0:4], e4[:], 1.0, 0.0, ALU.mult, ALU.add, ['e4'], ['junk32', ('smB5', pb_)],
                           accum_out=smB[:, pb_, 5:6])
                        sc.op('dve', (lambda e, pb_=pb_: e.reciprocal(smB[:, pb_, 6:7], smB[:, pb_, 5:6])),
                              r=[('smB5', pb_)], w=[('smB6', pb_)])
                        ts('dve', gate4[:, tile, :], e4[:], smB[:, pb_, 6:7], None, ALU.mult, None,
                           ['e4', ('smB6', pb_)], [('gate4', tile)])
                        act(x1b[pb_][:], X1c[:], AF.Copy, [kx], [('x1b', pb_)])
                        for jj in range(4):
                            sc.dma('pool', (lambda e, tile=tile, jj=jj, pb_=pb_: e.indirect_dma_start(
                                out=xs_d[:, :], out_offset=bass.IndirectOffsetOnAxis(slot4[:, tile, jj:jj + 1], 0),
                                in_=x1b[pb_][:], in_offset=None)), r=[('x1b', pb_), ('slot4', tile)],
                                w=[('xs', tile, jj)])


                    return [lambda: W_a(0), lambda: W_b(0), lambda: W_a(1), lambda: Tl_a(0), lambda: W_b(1),
                            lambda: Tl_b(0), lambda: W_a(2), lambda: Tl_a(1), lambda: W_b(2), lambda: Tl_b(1),
                            lambda: W_a(3), lambda: Tl_a(2), lambda: W_b(3), lambda: Tl_b(2), lambda: Tl_a(3),
                            lambda: Tl_b(3)]

                for u_ in front_units(0):
                    u_()
                for ci in range(NCI):
                    tu = tail_units(ci)
                    fu = front_units(ci + 1) if ci + 1 < NCI else []
                    nt, nf = len(tu), len(fu)
                    fi = 0
                    for k_, t_ in enumerate(tu):
                        t_()
                        want = ((k_ + 1) * nf) // nt
                        while fi < want:
                            fu[fi]()
                            fi += 1
                if dbg == 'cnt':
                    dma('sp', dbg_d[0:128, 0:32], carry[:], ['carry'], ['dbgcnt'])
                    sc.wait_keys('sp', ['dbgcnt'])
            sc.barrier()

        if 'C' in phases:
            pcs = ExitStack()
            with pcs:
                Wup = [sb('Wup%d' % i, [128, 8, 2048], BF16, pcs) for i in range(2)]
                Wdn = [sb('Wdn%d' % i, [128, 8, D], BF16, pcs) for i in range(2)]
                bdn = [sb('bdn%d' % i, [128, D], F32, pcs) for i in range(2)]
                bup = sb('bup', [128, 512], F32, pcs)
                dma('sp', bup[:], b_up_d[:, :], (), ['bup'])
                XsT = [sb('XsT%d' % i, [128, 8, C], BF16, pcs) for i in range(2)]
                hT = sb('hT', [128, 8, C], BF16, pcs)
                yst = [sb('yst%d' % i, [128, D], F32, pcs) for i in range(2)]
                NSW = 3
                sw = [[sb('sw%d_%d' % (i, k), [128, 512], F32, pcs) for k in range(3)] for i in range(NSW)]
                swn = [0]
                nbk = [0]

                def nb():
                    b = nbk[0] % 8
                    nbk[0] += 1
                    return b
                all_xs = [('xs', t, jj) for t in range(NT) for jj in range(4)]

                def load_w(e):
                    pe_ = e % 2
                    wu = w_up_d[e].rearrange("(kc p) n -> p kc n", p=128)
                    wd = w_dn_d[e].rearrange("(kc p) n -> p kc n", p=128)
                    if e == 0:
                        for fb in range(8):
                            for off_, kk_ in ((0, fb), (1024, 8 + fb)):
                                c0 = off_ + fb * 128
                                dma('pool', Wup[0][:, :, c0:c0 + 128], wu[:, :, c0:c0 + 128], (), [('Wup0f', kk_)])
                    else:
                        extra = [('Wup0f', k_) for k_ in range(16)] if e == 2 else []
                        for kc in range(8):
                            dma('pool', Wup[pe_][:, kc, :], wu[:, kc, :], (), [('Wup', pe_, kc)] + extra)
                    for kc in range(8):
                        dma('pool', Wdn[pe_][:, kc, :], wd[:, kc, :], (), [('Wdn', pe_, kc)])
                    dma('pool', bdn[pe_][:], b_dn_d[e:e + 1, :].to_broadcast([128, D]), (), [('bdn', pe_)])

                def xs_tload(e):
                    for kc in range(8):
                        sc.dma('sp', (lambda q, e=e, kc=kc: q.dma_start_transpose(
                            out=XsT[e % 2][:, kc, :], in_=xs_d[e * C:(e + 1) * C, kc * 128:(kc + 1) * 128])),
                            r=(all_xs if e == 0 else []), w=[('XsT', e % 2, kc)])

                slot_tiles = []
                o = 0
                while o < C:
                    n = min(512, C - o)
                    slot_tiles.append((o, n))
                    o += n

                pend_b = [None]

                def flush_b():
                    f_ = pend_b[0]
                    pend_b[0] = None
                    if f_ is not None:
                        f_()

                def up_unit(e, o, n, f):
                    pe_ = e % 2
                    bG, bL = nb(), nb()
                    for kc in range(8):
                        wk_ = ('Wup0f', f) if e == 0 else ('Wup', pe_, kc)
                        mm(banks[bG][:, 0:n], Wup[pe_][:, kc, f * 128:(f + 1) * 128], XsT[pe_][:, kc, o:o + n],
                           kc == 0, kc == 7, [wk_, ('XsT', pe_, kc)], [bk(bG)], signal=(kc == 7))
                    for kc in range(8):
                        wk_ = ('Wup0f', 8 + f) if e == 0 else ('Wup', pe_, kc)
                        mm(banks[bL][:, 0:n], Wup[pe_][:, kc, 1024 + f * 128:1024 + (f + 1) * 128],
                           XsT[pe_][:, kc, o:o + n], kc == 0, kc == 7, [wk_, ('XsT', pe_, kc)],
                           [bk(bL)], signal=(kc == 7))
                    si = swn[0] % NSW
                    swn[0] += 1
                    g1, sgt, l0 = sw[si]
                    kg1, ksg, kl0 = [('sw', si, k) for k in range(3)]
                    cg = e * 16 + f
                    cl = e * 16 + 8 + f
                    ts('dve', g1[:, 0:n], banks[bG][:, 0:n], bup[:, cg:cg + 1], 7.0, ALU.add, ALU.min,
                       ['bup'], [bk(bG), kg1])
                    act(sgt[:, 0:n], g1[:, 0:n], AF.Sigmoid, [kg1], [ksg], scale=1.702)
                    act(l0[:, 0:n], banks[bL][:, 0:n], AF.Identity, ['bup'], [bk(bL), kl0],
                        bias=bup[:, cl:cl + 1])
                    hks = [('hT', kb) for kb in range(o // 128, (o + n) // 128)]

                    def part_b(g1=g1, sgt=sgt, l0=l0, kg1=kg1, ksg=ksg, kl0=kl0, hks=hks, f=f, o=o, n=n):
                        ts('dve', l0[:, 0:n], l0[:, 0:n], 7.0, -7.0, ALU.min, ALU.max, [], [kl0])
                        tt('dve', sgt[:, 0:n], g1[:, 0:n], sgt[:, 0:n], ALU.mult, [kg1], [ksg])
                        stt(hT[:, f, o:o + n], l0[:, 0:n], 1.0, sgt[:, 0:n], ALU.add, ALU.mult, [kl0, ksg], hks)
                    flush_b()
                    pend_b[0] = part_b

                def down_blocks(e, o, n):
                    pe_ = e % 2
                    for sbk in range(o // 128, (o + n) // 128):
                        yb = yst[sbk % 2]
                        for half in range(2):
                            b = nb()
                            for fc in range(8):
                                mm(banks[b][:], hT[:, fc, sbk * 128:(sbk + 1) * 128],
                                   Wdn[pe_][:, fc, half * 512:(half + 1) * 512], fc == 0, fc == 7,
                                   [('hT', sbk), ('Wdn', pe_, fc)], [bk(b)], signal=(fc == 7))
                            hs = slice(half * 512, (half + 1) * 512)
                            tt('dve', yb[:, hs], banks[b][:], bdn[pe_][:, hs], ALU.add, [('bdn', pe_)],
                               [bk(b), ('yst', sbk % 2, half)])
                        r0 = e * C + sbk * 128
                        dma('sp', ys_d[r0:r0 + 128, :], yb[:], [('yst', sbk % 2, 0), ('yst', sbk % 2, 1)],
                            [('ys', e, sbk)])

                load_w(0)
                xs_tload(0)
                for e in range(NE):
                    if e + 1 < NE:
                        load_w(e + 1)
                        xs_tload(e + 1)
                    for si_, (o, n) in enumerate(slot_tiles):
                        for f in range(8):
                            up_unit(e, o, n, f)
                        if si_ >= 1:
                            down_blocks(e, *slot_tiles[si_ - 1])
                    flush_b()
                    down_blocks(e, *slot_tiles[-1])
            sc.barrier()

        if 'D' in phases:
            pds = ExitStack()
            with pds:
                ln2g = sb('ln2g', [128, D], F32, pds)
                ln2b = sb('ln2b', [128, D], F32, pds)
                dma('sp', ln2g[:], ln2g_d.to_broadcast([128, D]), (), ['ln2g'])
                dma('sp', ln2b[:], ln2b_d.to_broadcast([128, D]), (), ['ln2b'])
                ND = 6
                mhalfD = sb('mhalfD', [128, 4], F32, pds)
                memset('pool', mhalfD[:], -0.5, ['mhalfD'])
                yg = [[sb('yg%d_%d' % (i, jj), [128, D], F32, pds) for jj in range(4)] for i in range(ND)]
                bs = [sb('bsD%d' % i, [128, D], F32, pds) for i in range(ND)]
                stD = sb('stD', [128, ND, 12], F32, pds)
                smD = sb('smD', [128, ND, 8], F32, pds)
                all_ys = [('ys', e, sbk) for e in range(NE) for sbk in range(CB)]

                def d_load(t):
                    pt_ = t % ND
                    dma('sp', bs[pt_][:], base_d[t * 128:(t + 1) * 128, :], [('base', t)], [('bsD', pt_)])
                    for jj in range(4):
                        sc.dma('pool', (lambda e, t=t, jj=jj, pt_=pt_: e.indirect_dma_start(
                            out=yg[pt_][jj][:], out_offset=None, in_=ys_d[:, :],
                            in_offset=bass.IndirectOffsetOnAxis(slot4[:, t, jj:jj + 1], 0))),
                            r=(all_ys if t == 0 else []) + [('slot4', t)], w=[('yg', pt_, jj)])

                def d_part1(t):
                    pt_ = t % ND
                    y = yg[pt_]
                    kb = ('bsD', pt_)
                    for jj in range(4):
                        stt(bs[pt_][:], y[jj][:], gate4[:, t, jj:jj + 1], bs[pt_][:], ALU.mult, ALU.add,
                            [('yg', pt_, jj), ('gate4', t)], [kb])
                    src = bs[pt_]
                    sc.op('dve', (lambda e, src=src, pt_=pt_: e.bn_stats(stD[:, pt_, 0:6], src[:, 0:512])), r=[kb], w=[('stD0', pt_)])
                    sc.op('dve', (lambda e, src=src, pt_=pt_: e.bn_stats(stD[:, pt_, 6:12], src[:, 512:1024])), r=[kb], w=[('stD1', pt_)])
                    sc.op('dve', (lambda e, pt_=pt_: e.bn_aggr(smD[:, pt_, 0:2], stD[:, pt_, 0:12])),
                          r=[('stD0', pt_), ('stD1', pt_)], w=[('smD', pt_)])
                    ts('dve', smD[:, pt_, 2:3], smD[:, pt_, 1:2], 1.0, EPS, ALU.mult, ALU.add, [('smD', pt_)], [('smD2', pt_)])
                    tt('pool', smD[:, pt_, 3:4], smD[:, pt_, 2:3], mhalfD[:, 0:1], ALU.pow,
                       [('smD2', pt_), 'mhalfD'], [('smD3', pt_)])
                    ts('pool', smD[:, pt_, 4:5], smD[:, pt_, 0:1], smD[:, pt_, 3:4], -1.0, ALU.mult, ALU.mult,
                       [('smD', pt_), ('smD3', pt_)], [('smD4', pt_)])
                    act(y[0][:], bs[pt_][:], AF.Identity, [kb, ('smD3', pt_), ('smD4', pt_)], [('yg', pt_, 0)],
                        bias=smD[:, pt_, 4:5], scale=smD[:, pt_, 3:4])

                def d_part2(t):
                    pt_ = t % ND
                    y = yg[pt_]
                    tt('dve', y[0][:], y[0][:], ln2g[:], ALU.mult, ['ln2g'], [('yg', pt_, 0)])
                    tt('dve', y[0][:], y[0][:], ln2b[:], ALU.add, ['ln2b'], [('yg', pt_, 0)])
                    dma('sp', out_d[t * 128:(t + 1) * 128, :], y[0][:], [('yg', pt_, 0)], [('out', t)])

                PF = ND - 2
                for t in range(min(PF, NT)):
                    d_load(t)
                for t in range(NT):
                    if t + PF < NT:
                        d_load(t + PF)
                    d_part1(t)
                    if t >= 1:
                        d_part2(t - 1)
                d_part2(NT - 1)
            sc.wait_keys('sp', [('out', t) for t in range(NT)])

        if dbg == 'B':
            sc.wait_keys('sp', [('dbgout', t) for t in range(NT)])

        sc.wait_keys('sp', ['outfinal'])

        with nc.Block() as block:
            @block.tensor
            def _(e):
                sc.replay('pe', e)

            @block.scalar
            def _(e):
                sc.replay('act', e)

            @block.vector
            def _(e):
                sc.replay('dve', e)

            @block.gpsimd
            def _(e):
                sc.replay('pool', e)

            @block.sync
            def _(e):
                sc.replay('sp', e)
    return nc


def make_consts(S, C):
    NKT = S // 128
    ident = np.eye(128, dtype=np.float32)
    tri = (np.arange(128)[:, None] < np.arange(128)[None, :]).astype(np.float32)
    kp = np.arange(128)[:, None]
    qp = np.arange(128)[None, :]
    m1 = np.where(kp > qp, NEG, 0.0).astype(np.float32)
    mask = np.tile(m1, (1, 4))
    slopes = np.array([2.0 ** (-8.0 * (h + 1) / 4) for h in range(4)], dtype=np.float64)
    alibi = np.zeros((128, 4, NKT), dtype=np.float64)
    for h in range(4):
        for dd in range(NKT):
            alibi[:, h, dd] = slopes[h] * (np.arange(128) - 128.0 * dd)
    alibi = alibi.reshape(128, 4 * NKT).astype(np.float32)
    ratio = np.zeros((128, 4, 16), dtype=np.float32)
    for g, w in enumerate((2, 4, 8, 16)):
        for t in range(16):
            ratio[:, g, t] = float(w) / float(min(t + 1, w))
    ratio = ratio.reshape(128, 64)
    carry0 = np.tile((np.arange(32, dtype=np.float32) * C)[None, :], (128, 1))
    band = np.zeros((128, 4, 3, 128), dtype=np.float64)
    tp_ = np.arange(128)[:, None]
    t_ = np.arange(128)[None, :]
    for g, w in enumerate((2, 4, 8, 16)):
        dlt = t_ - tp_
        band[:, g, 0, :] = np.where((dlt >= 0) & (dlt < w), 1.0 / w, 0.0) - (dlt == 0)
        band[:, g, 1, :] = np.where((dlt + 128 > 0) & (dlt + 128 < w), 1.0 / w, 0.0)
        cntt = np.minimum(t_ + 1, w).astype(np.float64)
        band[:, g, 2, :] = np.where((dlt >= 0) & (dlt < w), 1.0 / cntt, 0.0) - (dlt == 0)
    band = band.reshape(128, 4 * 3 * 128).astype(np.float32)
    return dict(c_ident=ident, c_tri=tri, c_mask=mask, c_alibi=alibi, c_ratio=ratio, c_carry0=carry0, c_band=band)


def make_in_maps(inputs, cfg, ncores):
    S, NSEQ, C = cfg['S'], cfg['NSEQ'], cfg['C']
    f = lambda a: np.ascontiguousarray(np.asarray(a, dtype=np.float32))
    x = f(inputs['x'])
    p = f(inputs['p'])[0]
    shared = dict(
        w_in=f(inputs['w_in'])[0],
        b_gate=f(f(inputs['b_gate'])[0].reshape(16, 128).T),
        lam=f(np.concatenate([np.asarray(inputs[k], dtype=np.float32)[0] for k in
                              ('lambda_q1', 'lambda_k1', 'lambda_q2', 'lambda_k2')]).reshape(1, 256)),
        subln_g=f(inputs['subln_g']).reshape(1, 128),
        w_attn_br=f(inputs['w_attn_br'])[0],
        w_pool_mix=f(inputs['w_pool_mix'])[0],
        pool_scale=f(f(inputs['pool_scale'])[0].reshape(4, 128).T),
        w_pool_br=f(inputs['w_pool_br'])[0],
        w_out=f(inputs['w_out'])[0],
        ln1_g=f(inputs['ln1_g']).reshape(1, D),
        ln1_b=f(inputs['ln1_b']).reshape(1, D),
        w_router=f(inputs['w_router'])[0],
        b_router=f(inputs['b_router']).reshape(1, 32),
        w_up=f(inputs['w_up'])[0],
        b_up=f(f(inputs['b_up'])[0].reshape(32, 16, 128).transpose(2, 0, 1).reshape(128, 512)),
        w_down=f(inputs['w_down'])[0],
        b_down=f(inputs['b_down'])[0],
        w_ple_gate=f(inputs['w_ple_gate'])[0],
        w_ple_proj=f(inputs['w_ple_proj'])[0],
        ln2_g=f(inputs['ln2_g']).reshape(1, D),
        ln2_b=f(inputs['ln2_b']).reshape(1, D),
    )
    shared.update(make_consts(S, C))
    maps = []
    for c in range(ncores):
        m = dict(shared)
        m['x'] = f(x[c * NSEQ:(c + 1) * NSEQ].reshape(NSEQ * S, D))
        m['p'] = f(p[c * NSEQ:(c + 1) * NSEQ].reshape(NSEQ * S, 256))
        maps.append(m)
    return maps


FULL_CFG = dict(S=4096, NSEQ=2, C=1280)


def kernel(**inputs):
    cfg = dict(FULL_CFG)
    nc = build(cfg)
    maps = make_in_maps(inputs, cfg, NCORES)
    res = run_bass_kernel_spmd(nc, maps, core_ids=list(range(NCORES)))
    outs = [np.asarray(r["out"], dtype=np.float32).reshape(cfg['NSEQ'], cfg['S'], D) for r in res.results]
    return np.concatenate(outs, axis=0)
```
